# Optimizing a Trainium2 kernel written in Bass

```python
import jax, jax.numpy as jnp
from jax import lax
import numpy as np

D_MODEL = 1024
BATCH = 4
SEQ = 8192
DEPTH = 1

D_MIX = D_MODEL
HEAD_DIM = 64
MOBA_HEADS = (D_MIX // 2) // HEAD_DIM
MOBA_WIDTH = MOBA_HEADS * HEAD_DIM
DIFF_HEADS = (D_MIX // 2) // (2 * HEAD_DIM)
DIFF_WIDTH = DIFF_HEADS * 2 * HEAD_DIM
MOBA_BLOCK = 256
MOBA_TOPK = 3
MOBA_Q_CHUNK = 64
DIFF_Q_CHUNK = 128
IN_COLS = 3 * MOBA_WIDTH + 3 * DIFF_WIDTH + D_MIX
EPS = 1e-6

kernel_name = "moba_diffattn_parallel_hybrid_adaln"


def rmsnorm(x, g):
    xf = x.astype(jnp.float32)
    y = xf * lax.rsqrt(jnp.mean(xf * xf, axis=-1, keepdims=True) + EPS)
    return (y * g.astype(jnp.float32)).astype(x.dtype)


def alibi_slopes(n):
    return jnp.asarray(2.0 ** (-8.0 * np.arange(1, n + 1, dtype=np.float32) / n), dtype=jnp.float32)


def moba_attention(q, k, v, slopes):
    B, H, S, Dh = q.shape
    s_pad = ((S + MOBA_BLOCK - 1) // MOBA_BLOCK) * MOBA_BLOCK
    pad = [(0, 0), (0, 0), (0, s_pad - S), (0, 0)]
    k_pad = jnp.pad(k, pad)
    v_pad = jnp.pad(v, pad)
    nb = s_pad // MOBA_BLOCK
    kk = min(MOBA_TOPK, nb)
    k_blk = k_pad.reshape(B, H, nb, MOBA_BLOCK, Dh)
    v_blk = v_pad.reshape(B, H, nb, MOBA_BLOCK, Dh)
    k_mean = jnp.mean(k_blk.astype(jnp.float32), axis=3)
    scale = Dh ** -0.5
    n_chunks = S // MOBA_Q_CHUNK
    q_chunks = q.reshape(B, H, n_chunks, MOBA_Q_CHUNK, Dh).transpose(2, 0, 1, 3, 4)
    bi = jnp.arange(B)[:, None, None, None]
    hi = jnp.arange(H)[None, :, None, None]
    blk_off = jnp.arange(MOBA_BLOCK)
    blk_ids = jnp.arange(nb)
    m = slopes[None, :, None, None]

    def chunk_fn(args):
        ci, qc = args
        start = ci * MOBA_Q_CHUNK
        t = start + jnp.arange(MOBA_Q_CHUNK)
        cur = start // MOBA_BLOCK
        qf = qc.astype(jnp.float32)
        gate = jnp.einsum('bhcd,bhnd->bhcn', qf, k_mean)
        gate = jnp.where(blk_ids < cur, gate, -jnp.inf)
        _, idx = lax.top_k(gate, kk)
        valid = idx < cur
        ks = k_blk[bi, hi, idx].astype(jnp.float32)
        vs = v_blk[bi, hi, idx].astype(jnp.float32)
        s_sel = jnp.einsum('bhcd,bhcjkd->bhcjk', qf, ks) * scale
        pos_sel = idx[..., None] * MOBA_BLOCK + blk_off
        s_sel = s_sel - m[..., None] * (t[None, None, :, None, None] - pos_sel).astype(jnp.float32)
        s_sel = jnp.where(valid[..., None], s_sel, -jnp.inf)
        k_own = lax.dynamic_slice_in_dim(k_pad, cur * MOBA_BLOCK, MOBA_BLOCK, axis=2).astype(jnp.float32)
        v_own = lax.dynamic_slice_in_dim(v_pad, cur * MOBA_BLOCK, MOBA_BLOCK, axis=2).astype(jnp.float32)
        s_own = jnp.einsum('bhcd,bhkd->bhck', qf, k_own) * scale
        dist_own = t[:, None] - (cur * MOBA_BLOCK + blk_off)[None, :]
        s_own = s_own - m * dist_own.astype(jnp.float32)
        s_own = jnp.where(dist_own >= 0, s_own, -jnp.inf)
        logits = jnp.concatenate([s_sel.reshape(B, H, MOBA_Q_CHUNK, kk * MOBA_BLOCK), s_own], axis=-1)
        p = jax.nn.softmax(logits, axis=-1)
        p_sel = p[..., :kk * MOBA_BLOCK].reshape(B, H, MOBA_Q_CHUNK, kk, MOBA_BLOCK)
        p_own = p[..., kk * MOBA_BLOCK:]
        out = jnp.einsum('bhcjk,bhcjkd->bhcd', p_sel, vs) + jnp.einsum('bhck,bhkd->bhcd', p_own, v_own)
        return out.astype(q.dtype)

    out = lax.map(chunk_fn, (jnp.arange(n_chunks), q_chunks))
    return out.transpose(1, 2, 0, 3, 4).reshape(B, H, S, Dh)


def diff_attention(q, k, v, lam, lam_init, subln_g, slopes):
    B, H, _, S, Dh = q.shape
    scale = Dh ** -0.5
    n_chunks = S // DIFF_Q_CHUNK
    q_chunks = q.reshape(B, H, 2, n_chunks, DIFF_Q_CHUNK, Dh).transpose(3, 0, 1, 2, 4, 5)
    kf = k.astype(jnp.float32)
    vf = v.astype(jnp.float32)
    s_pos = jnp.arange(S)
    m = slopes[None, :, None, None, None]

    def chunk_fn(args):
        ci, qc = args
        t = ci * DIFF_Q_CHUNK + jnp.arange(DIFF_Q_CHUNK)
        s = jnp.einsum('bhicd,bhisd->bhics', qc.astype(jnp.float32), kf) * scale
        dist = t[:, None] - s_pos[None, :]
        s = s - m * dist.astype(jnp.float32)
        s = jnp.where(dist >= 0, s, -jnp.inf)
        p = jax.nn.softmax(s, axis=-1)
        a = p[:, :, 0] - lam * p[:, :, 1]
        return jnp.einsum('bhcs,bhse->bhce', a, vf)

    out = lax.map(chunk_fn, (jnp.arange(n_chunks), q_chunks))
    out = out.transpose(1, 2, 0, 3, 4).reshape(B, H, S, 2 * Dh)
    out = rmsnorm(out, subln_g) * (1.0 - lam_init)
    return out.astype(q.dtype)


def setup_inputs(seed: int = 0) -> dict:
    key = jax.random.key(seed)
    ks = jax.random.split(key, 14)
    f32 = jnp.float32
    x = jax.random.normal(ks[0], (BATCH, SEQ, D_MODEL), f32)
    c = jax.random.normal(ks[1], (BATCH, D_MODEL), f32)
    ln_g = 1.0 + 0.02 * jax.random.normal(ks[2], (DEPTH, D_MODEL), f32)
    w_ada = jax.random.normal(ks[3], (DEPTH, D_MODEL, 3 * D_MODEL), f32) * D_MODEL ** -0.5
    b_ada = 0.02 * jax.random.normal(ks[4], (DEPTH, 3 * D_MODEL), f32)
    w_in = jax.random.normal(ks[5], (DEPTH, D_MODEL, IN_COLS), f32) * D_MODEL ** -0.5
    lambda_q1 = 0.1 * jax.random.normal(ks[6], (DEPTH, HEAD_DIM), f32)
    lambda_k1 = 0.1 * jax.random.normal(ks[7], (DEPTH, HEAD_DIM), f32)
    lambda_q2 = 0.1 * jax.random.normal(ks[8], (DEPTH, HEAD_DIM), f32)
    lambda_k2 = 0.1 * jax.random.normal(ks[9], (DEPTH, HEAD_DIM), f32)
    subln_g = 1.0 + 0.02 * jax.random.normal(ks[10], (DEPTH, 2 * HEAD_DIM), f32)
    w_out = jax.random.normal(ks[11], (DEPTH, D_MIX, D_MODEL), f32) * D_MIX ** -0.5
    final_g = 1.0 + 0.02 * jax.random.normal(ks[12], (D_MODEL,), f32)
    return {"x": x, "c": c, "ln_g": ln_g, "w_ada": w_ada, "b_ada": b_ada, "w_in": w_in,
            "lambda_q1": lambda_q1, "lambda_k1": lambda_k1, "lambda_q2": lambda_q2, "lambda_k2": lambda_k2,
            "subln_g": subln_g, "w_out": w_out, "final_g": final_g}


def reference(x, c, ln_g, w_ada, b_ada, w_in, lambda_q1, lambda_k1, lambda_q2, lambda_k2,
              subln_g, w_out, final_g):
    B, S, D = x.shape
    moba_slopes = alibi_slopes(MOBA_HEADS)
    diff_slopes = alibi_slopes(DIFF_HEADS)
    c_act = jax.nn.silu(c)
    for l in range(DEPTH):
        mod = c_act @ w_ada[l] + b_ada[l]
        shift, scl, gate = jnp.split(mod, 3, axis=-1)
        h = rmsnorm(x, ln_g[l]) * (1.0 + scl[:, None, :]) + shift[:, None, :]
        proj = h @ w_in[l]
        sizes = np.cumsum([MOBA_WIDTH] * 3 + [DIFF_WIDTH] * 3)
        mq, mk, mv, dq, dk, dv, z = jnp.split(proj, sizes, axis=-1)
        to_heads = lambda a, H: a.reshape(B, S, H, -1).transpose(0, 2, 1, 3)
        a_out = moba_attention(to_heads(mq, MOBA_HEADS), to_heads(mk, MOBA_HEADS),
                               to_heads(mv, MOBA_HEADS), moba_slopes)
        a_out = a_out.transpose(0, 2, 1, 3).reshape(B, S, MOBA_WIDTH)
        dq_h = dq.reshape(B, S, DIFF_HEADS, 2, HEAD_DIM).transpose(0, 2, 3, 1, 4)
        dk_h = dk.reshape(B, S, DIFF_HEADS, 2, HEAD_DIM).transpose(0, 2, 3, 1, 4)
        dv_h = to_heads(dv, DIFF_HEADS)
        lam_init = 0.8 - 0.6 * float(np.exp(-0.3 * l))
        lam = (jnp.exp(jnp.sum(lambda_q1[l].astype(jnp.float32) * lambda_k1[l].astype(jnp.float32)))
               - jnp.exp(jnp.sum(lambda_q2[l].astype(jnp.float32) * lambda_k2[l].astype(jnp.float32)))
               + lam_init)
        b_out = diff_attention(dq_h, dk_h, dv_h, lam, lam_init, subln_g[l], diff_slopes)
        b_out = b_out.transpose(0, 2, 1, 3).reshape(B, S, DIFF_WIDTH)
        y = jnp.concatenate([a_out, b_out], axis=-1) * jax.nn.silu(z)
        x = x + gate[:, None, :] * (y @ w_out[l])
    return rmsnorm(x, final_g)
```

```python
import numpy as np
import ml_dtypes
from contextlib import ExitStack
import concourse.bass as bass
import concourse.mybir as mybir
from concourse.alu_op_type import AluOpType as ALU
from concourse.bass_utils import run_bass_kernel_spmd

F32 = mybir.dt.float32
BF16 = mybir.dt.bfloat16
AF = mybir.ActivationFunctionType
AX = mybir.AxisListType
BF = ml_dtypes.bfloat16

S = 8192
D = 1024
NTA = 16
NTO = 8
EPS = 1e-6
NEG = -float(2 ** 18)
KROWS = 100
MOBA_SLOPES = [2.0 ** (-8.0 * (i + 1) / 8) for i in range(8)]
DIFF_SLOPES = [2.0 ** (-8.0 * (i + 1) / 4) for i in range(4)]
LAM_INIT = 0.8 - 0.6 * 1.0
ALIBI_THR = 40.0
DEBUG = False
gate_free = [None]
junk_tok = [None]


class Sem:
    def __init__(self, nc, es, name):
        self.h = es.enter_context(nc.semaphore(name))
        self.n = 0
        self.name = name


class Ctx:
    def __init__(self, nc, es):
        self.nc = nc
        self.es = es
        self.waited = {}
        self.nsem = 0
        self.E = {"pe": nc.tensor, "act": nc.scalar, "dve": nc.vector, "pool": nc.gpsimd, "sp": nc.sync}
        self.prog = {}
        self.last = {}
        self.dma_toks = []
        self.bar = self.sem("bar")
        self.new_prog()

    def sem(self, name):
        self.nsem += 1
        return Sem(self.nc, self.es, f"{name}_{self.nsem}")

    def new_prog(self):
        for k in ["pe", "act", "dve", "pool"]:
            self.prog[k] = self.sem(k + "_prog")

    def wait(self, eng, *toks):
        for t in toks:
            if t is None:
                continue
            if isinstance(t, list):
                self.wait(eng, *t)
                continue
            sem, val = t
            key = (eng, sem.name)
            if self.waited.get(key, 0) >= val:
                continue
            self.E[eng].wait_ge(sem.h, val)
            self.waited[key] = val

    def sig(self, eng, inst):
        sem = self.prog[eng]
        inst.then_inc(sem.h, 1)
        sem.n += 1
        tok = (sem, sem.n)
        self.last[eng] = tok
        return tok

    def dma(self, eng, out, in_, sem, **kw):
        inst = self.E[eng].dma_start(out=out, in_=in_, **kw)
        inst.then_inc(sem.h, 16)
        sem.n += 16
        tok = (sem, sem.n)
        self.dma_toks.append(tok)
        return tok

    def final_dma_toks(self):
        best = {}
        for sem, val in self.dma_toks:
            if sem.name not in best or best[sem.name][1] < val:
                best[sem.name] = (sem, val)
        return list(best.values())

    def barrier(self):
        toks = list(self.last.values()) + self.final_dma_toks()
        self.wait("sp", *toks)
        self.nc.sync.sem_inc(self.bar.h, 1)
        self.bar.n += 1
        for e in ["pe", "act", "dve", "pool"]:
            self.E[e].wait_ge(self.bar.h, self.bar.n)
        self.dma_toks = []
        self.last = {}
        self.new_prog()


def bcast_rows(handle, n):
    return bass.AP(handle, 0, [[0, 128], [1, n]])


def build_program():
    gate_free[0] = None
    junk_tok[0] = None
    nc = bass.Bass("TRN2", target_bir_lowering=False)
    dt = nc.dram_tensor
    x_all = dt("x_all", [S, D], F32, kind="ExternalInput").ap()
    x_own = dt("x_own", [S // 2, D], F32, kind="ExternalInput").ap()
    c_t = dt("c_t", [128, 8], F32, kind="ExternalInput").ap()
    ln_g = dt("ln_g", [1, D], F32, kind="ExternalInput")
    b_ada = dt("b_ada", [1, 3 * D], F32, kind="ExternalInput")
    final_g = dt("final_g", [1, D], F32, kind="ExternalInput")
    subln_g = dt("subln_g", [1, 128], F32, kind="ExternalInput")
    lams = [dt(n, [1, 64], F32, kind="ExternalInput") for n in ("lambda_q1", "lambda_k1", "lambda_q2", "lambda_k2")]
    w_ada = dt("w_ada", [D, 3 * D], F32, kind="ExternalInput").ap()
    w_in = dt("w_in", [D, 4 * D], F32, kind="ExternalInput").ap()
    w_out = dt("w_out", [D, D], F32, kind="ExternalInput").ap()
    ident_d = dt("ident", [128, 128], BF16, kind="ExternalInput").ap()
    kaug_d = dt("kaug", [36, S], BF16, kind="ExternalInput").ap()
    qaug_d = dt("qaug", [12, 4, S // 2], BF16, kind="ExternalInput").ap()
    cmask_d = dt("cmask", [128, 128], BF16, kind="ExternalInput").ap()
    gconst_d = dt("gconst", [128, 3, 32, 32], F32, kind="ExternalInput").ap()
    zrows_d = dt("zrows", [32, S // 2], BF16, kind="ExternalInput").ap()
    out_d = dt("out", [S // 2, D], F32, kind="ExternalOutput").ap()
    skind = "ExternalOutput" if DEBUG else "Internal"
    KT_scr = dt("KT_scr", [8, 128, S], BF16, kind=skind).ap()
    VM_scr = dt("VM_scr", [8, 128, 64, 65], BF16, kind=skind).ap()
    VD_scr = dt("VD_scr", [4, 128, 64, 129], BF16, kind=skind).ap()
    QT_scr = dt("QT_scr", [8, 128, S // 2], BF16, kind=skind).ap()
    QM_scr = dt("QM_scr", [256, S // 2], BF16, kind=skind).ap()
    att_scr = dt("att_scr", [S // 2, D], BF16, kind=skind).ap()

    w_in_v = w_in.rearrange("(kc p) c -> p kc c", p=128)
    w_ada_v = w_ada.rearrange("(kc p) c -> p kc c", p=128)
    w_out_v = w_out.rearrange("(kc p) c -> p kc c", p=128)

    with ExitStack() as es:
        cx = Ctx(nc, es)
        sb = lambda name, shape, dtype: es.enter_context(nc.sbuf_tensor(name, shape, dtype))
        psum = [es.enter_context(nc.psum_tensor(f"ps{i}", [128, 512], F32)) for i in range(8)]
        ident = sb("ident_sb", [128, 128], BF16)
        mod_bc = sb("mod_bc", [128, 3 * D], F32)
        fing_bc = sb("fing_bc", [128, D], F32)
        subg_bc = sb("subg_bc", [128, 128], F32)
        neglam = sb("neglam", [128, 1], F32)
        mhalf = sb("mhalf", [128, 1], F32)
        ones_bf = sb("ones_bf", [128, 1], BF16)
        junk = sb("junk", [128, D], BF16)
        hs_sb = sb("hs_sb", [128, 8, 32], F32)
        shift_bc = mod_bc[:, 0:D]
        geff_bc = mod_bc[:, D:2 * D]
        gate_bc = mod_bc[:, 2 * D:3 * D]
        s_c = cx.sem("ld_const")

        t_ident = cx.dma("sp", ident[:], ident_d[:, :], s_c)
        t_fing = cx.dma("sp", fing_bc[:], bcast_rows(final_g, D), cx.sem("ld_fing"))
        t_subg = cx.dma("sp", subg_bc[:], bcast_rows(subln_g, 128), cx.sem("ld_subg"))
        cx.sig("pool", nc.gpsimd.memset(mhalf[:], -0.5))
        t_ones = cx.sig("pool", nc.gpsimd.memset(ones_bf[:], 1.0))

        p12 = ExitStack()
        wkv = p12.enter_context(nc.sbuf_tensor("wkv_pre", [128, 8, 2048], BF16))
        wq = p12.enter_context(nc.sbuf_tensor("wq_pre", [128, 8, 1536], BF16))
        s_wkv = cx.sem("ld_wkv")
        s_wq = cx.sem("ld_wq")
        for i, c0 in enumerate([512, 2048, 1024, 2560]):
            t_wkv = cx.dma("pool", wkv[:, :, i * 512:(i + 1) * 512], w_in_v[:, :, c0:c0 + 512], s_wkv)
        for i, c0 in enumerate([0, 1536, 512]):
            t_wq = cx.dma("pool", wq[:, :, i * 512:(i + 1) * 512], w_in_v[:, :, c0:c0 + 512], s_wq)
        with ExitStack() as p0:
            sb0 = lambda name, shape, dtype: p0.enter_context(nc.sbuf_tensor('p0_' + name, shape, dtype))
            ct = sb0("ct", [128, 8], F32)
            cact = sb0("cact", [128, 8], F32)
            sg = sb0("sg", [128, 8], F32)
            ones_f = sb0("ones_f", [128, 128], F32)
            crep = sb0("crep", [128, 8, 128], F32)
            bada_bc = sb0("bada_bc", [128, 3 * D], F32)
            lng_bc = sb0("lng_bc", [128, D], F32)
            wa = [sb0(f"wa{i}", [128, 8, 512], F32) for i in range(6)]
            lam_t = [sb0(f"lam{i}", [128, 64], F32) for i in range(4)]
            lprod = sb0("lprod", [128, 2, 64], F32)
            lsum = sb0("lsum", [128, 2], F32)
            lexp = sb0("lexp", [128, 2], F32)
            s_p0 = cx.sem("ld_p0")
            s_wa = [cx.sem("ld_wa") for _ in range(6)]
            t_ct = cx.dma("sp", ct[:], c_t[:, :], s_p0)
            t_bada = cx.dma("sp", bada_bc[:], bcast_rows(b_ada, 3 * D), s_p0)
            t_lng = cx.dma("sp", lng_bc[:], bcast_rows(ln_g, D), s_p0)
            for i in range(4):
                t_lam = cx.dma("sp", lam_t[i][:], bcast_rows(lams[i], 64), s_p0)
            t_p0 = t_lam
            cx.wait("act", t_p0)
            t = cx.sig("act", nc.scalar.activation(out=sg[:], in_=ct[:], func=AF.Sigmoid))
            t_of = cx.sig("dve", nc.vector.memset(ones_f[:], 1.0))
            cx.wait("dve", t, t_p0, t_of)
            t = cx.sig("dve", nc.vector.tensor_tensor(out=cact[:], in0=ct[:], in1=sg[:], op=ALU.mult))
            cx.wait("dve", t)
            for kc in range(8):
                t_crep = cx.sig("dve", nc.vector.tensor_scalar(out=crep[:, kc, :], in0=ones_f[:], scalar1=cact[:, kc:kc + 1],
                                                               scalar2=None, op0=ALU.mult))
            ps_free = [None, None]
            t_was = [cx.dma("sp", wa[cg][:], w_ada_v[:, :, cg * 512:(cg + 1) * 512], s_wa[cg]) for cg in range(6)]
            for cg in range(6):
                b = cg % 2
                cx.wait("pe", t_was[cg], t_crep, ps_free[b])
                for kc in range(8):
                    mm = nc.tensor.matmul(psum[b][:, :], lhsT=crep[:, kc, :], rhs=wa[cg][:, kc, :], start=(kc == 0), stop=(kc == 7))
                t_mm = cx.sig("pe", mm)
                cx.wait("dve", t_mm, t_p0)
                t_ev = cx.sig("dve", nc.vector.tensor_tensor(out=mod_bc[:, cg * 512:(cg + 1) * 512], in0=psum[b][:, :],
                                                             in1=bada_bc[:, cg * 512:(cg + 1) * 512], op=ALU.add))
                ps_free[b] = t_ev
            cx.wait("dve", t_ev)
            t = cx.sig("dve", nc.vector.scalar_tensor_tensor(out=geff_bc, in0=geff_bc, scalar=1.0, in1=lng_bc[:], op0=ALU.add, op1=ALU.mult))
            t1 = cx.sig("dve", nc.vector.tensor_tensor(out=lprod[:, 0, :], in0=lam_t[0][:], in1=lam_t[1][:], op=ALU.mult))
            t2 = cx.sig("dve", nc.vector.tensor_tensor(out=lprod[:, 1, :], in0=lam_t[2][:], in1=lam_t[3][:], op=ALU.mult))
            cx.wait("dve", t2)
            t = cx.sig("dve", nc.vector.tensor_reduce(out=lsum[:], in_=lprod[:], axis=AX.X, op=ALU.add))
            cx.wait("act", t)
            t = cx.sig("act", nc.scalar.activation(out=lexp[:], in_=lsum[:], func=AF.Exp))
            cx.wait("dve", t)
            t = cx.sig("dve", nc.vector.tensor_tensor(out=neglam[:], in0=lexp[:, 1:2], in1=lexp[:, 0:1], op=ALU.subtract))
            cx.wait("dve", t)
            t = cx.sig("dve", nc.vector.tensor_scalar(out=neglam[:], in0=neglam[:], scalar1=-LAM_INIT, scalar2=None, op0=ALU.add))
            cx.wait("dve", t_subg)
            t = cx.sig("dve", nc.vector.tensor_scalar(out=subg_bc[:], in0=subg_bc[:], scalar1=(1.0 - LAM_INIT), scalar2=None, op0=ALU.mult))
            cx.barrier()

        tp_views = {}
        for bk in (4, 5, 6, 7):
            tp_views[bk] = psum[bk][:, :].bitcast(BF16)
        tp_free = {0: None, 1: None}

        class Pipe:
            def __init__(self, alloc, x_v, ntiles, nx=3):
                self.x_v = x_v
                self.nt = ntiles
                self.nx = nx
                self.xb = [alloc(f"xb{i}", [128, 4, D], F32) for i in range(nx)]
                self.hb = [alloc(f"hb{i}", [128, 4, D], BF16) for i in range(2)]
                self.hT = [alloc(f"hT{i}", [128, 8, 512], BF16) for i in range(2)]
                self.tmp = [alloc(f"tmp{i}", [128, D], F32) for i in range(2)]
                self.ssq = alloc("ssq", [128, 4 * ntiles], F32)
                self.rstd = alloc("rstd", [128, 4 * ntiles], F32)
                self.s_x = [cx.sem("ld_x") for _ in range(nx)]
                self.x_free = [None] * nx
                self.hb_free = [None] * 2
                self.hT_free = [None] * 2
                self.t_x, self.t_r, self.t_h, self.t_hT = {}, {}, {}, {}

            def L(self, T):
                if T >= self.nt:
                    return
                b = T % self.nx
                cx.wait("sp", self.x_free[b])
                self.t_x[T] = cx.dma("sp", self.xb[b][:], self.x_v[T], self.s_x[b])

            def Q(self, T):
                if T >= self.nt:
                    return
                xb = self.xb[T % self.nx]
                toks = []
                cx.wait("act", self.t_x[T])
                for c in range(4):
                    col = T * 4 + c
                    cx.wait("act", junk_tok[0])
                    t_ss = cx.sig("act", nc.scalar.activation(out=junk[:], in_=xb[:, c, :], func=AF.Square, accum_out=self.ssq[:, col:col + 1]))
                    junk_tok[0] = t_ss
                    cx.wait("pool", t_ss)
                    t_v = cx.sig("pool", nc.gpsimd.tensor_scalar(out=self.rstd[:, col:col + 1], in0=self.ssq[:, col:col + 1], scalar1=1.0 / D, scalar2=EPS,
                                                                 op0=ALU.mult, op1=ALU.add))
                    cx.wait("pool", t_v)
                    toks.append(cx.sig("pool", nc.gpsimd.tensor_tensor(out=self.rstd[:, col:col + 1], in0=self.rstd[:, col:col + 1], in1=mhalf[:], op=ALU.pow)))
                self.t_r[T] = toks

            def H(self, T):
                if T >= self.nt:
                    return
                xb = self.xb[T % self.nx]
                hb = self.hb[T % 2]
                toks = []
                cx.wait("dve", self.t_x[T], self.hb_free[T % 2])
                for c in range(4):
                    col = T * 4 + c
                    cx.wait("dve", self.t_r[T][c])
                    t_a = cx.sig("dve", nc.vector.scalar_tensor_tensor(out=self.tmp[c % 2][:], in0=xb[:, c, :], scalar=self.rstd[:, col:col + 1], in1=geff_bc,
                                                                       op0=ALU.mult, op1=ALU.mult))
                    cx.wait("dve", t_a)
                    toks.append(cx.sig("dve", nc.vector.tensor_tensor(out=hb[:, c, :], in0=self.tmp[c % 2][:], in1=shift_bc, op=ALU.add)))
                self.t_h[T] = toks
                self.x_free[T % self.nx] = toks[-1]

            def R(self, T):
                if T >= self.nt:
                    return
                hb = self.hb[T % 2]
                hT = self.hT[T % 2]
                toks = []
                for c in range(4):
                    bk = (6, 7)[c % 2]
                    tp = tp_views[bk]
                    cx.wait("pe", self.t_h[T][c], tp_free[c % 2], t_ident)
                    for kc in range(8):
                        tr = nc.tensor.transpose(tp[:, kc * 128:(kc + 1) * 128], hb[:, c, kc * 128:(kc + 1) * 128], ident[:])
                    t_tp = cx.sig("pe", tr)
                    cx.wait("act", t_tp, self.hT_free[T % 2])
                    t_e = cx.sig("act", nc.scalar.copy(out=hT[:, :, c * 128:(c + 1) * 128], in_=tp[:, :].rearrange("p (k t) -> p k t", k=8)))
                    tp_free[c % 2] = t_e
                    toks.append(t_e)
                self.t_hT[T] = toks
                self.hb_free[T % 2] = t_tp

        with ExitStack() as p1:
            sb1 = lambda name, shape, dtype: p1.enter_context(nc.sbuf_tensor('p1_' + name, shape, dtype))
            kst = [sb1(f"kst{i}", [128, 8, 512], BF16) for i in range(2)]
            vmst = [sb1(f"vmst{i}", [128, 8, 4, 65], BF16) for i in range(2)]
            vdst = [sb1(f"vdst{i}", [128, 4, 4, 129], BF16) for i in range(2)]
            s_kst = [cx.sem("st_k") for _ in range(2)]
            s_vst = [cx.sem("st_v") for _ in range(2)]
            t_w = t_wkv
            pre = []
            for i in range(2):
                t1 = cx.sig("dve", nc.vector.memset(vmst[i][:], 1.0))
                t2 = cx.sig("dve", nc.vector.memset(vdst[i][:], 1.0))
                pre = [t1, t2]
            x_all_v = x_all.rearrange("(t c p) d -> t p c d", c=4, p=128)
            pipe = Pipe(sb1, x_all_v, NTA)
            mm_free = [None] * 4
            kst_free = [None, None]
            vst_free = [None, None]
            mmi = [0]
            t_hs = [None]

            def R1(T):
                if T >= NTA:
                    return
                pipe.R(T)
                hb = pipe.hb[T % 2]
                cx.wait("pe", *pipe.t_h[T], t_ones)
                for blk in range(2):
                    for kc in range(8):
                        col = kc * 32 + 2 * T + blk
                        nc.tensor.matmul(psum[5][:, col:col + 1], lhsT=hb[:, 2 * blk, kc * 128:(kc + 1) * 128], rhs=ones_bf[:, 0:1],
                                         start=True, stop=False, skip_group_check=True)
                        mm = nc.tensor.matmul(psum[5][:, col:col + 1], lhsT=hb[:, 2 * blk + 1, kc * 128:(kc + 1) * 128], rhs=ones_bf[:, 0:1],
                                              start=False, stop=True, skip_group_check=True)
                t_hs[0] = cx.sig("pe", mm)
                pipe.hb_free[T % 2] = t_hs[0]

            def M1(T):
                b = T % 2
                hT = pipe.hT[b]
                cx.wait("pe", *pipe.t_hT[T], t_w)
                cx.wait("act", kst_free[b])
                for j in range(8):
                    pb = mmi[0] % 4
                    cx.wait("pe", mm_free[pb])
                    for kc in range(8):
                        mm = nc.tensor.matmul(psum[pb][:, :], lhsT=wkv[:, kc, j * 128:(j + 1) * 128], rhs=hT[:, kc, :], start=(kc == 0), stop=(kc == 7))
                    t_mm = cx.sig("pe", mm)
                    cx.wait("act", t_mm)
                    t_ev = cx.sig("act", nc.scalar.copy(out=kst[b][:, j, :], in_=psum[pb][:, :]))
                    mm_free[pb] = t_ev
                    mmi[0] += 1
                cx.wait("sp", t_ev)
                kst_free[b] = cx.dma("sp", KT_scr[:, :, T * 512:(T + 1) * 512].rearrange("j p t -> p j t"), kst[b][:], s_kst[b])
                cx.wait("act", vst_free[b], *pre)
                cx.wait("dve", vst_free[b], *pre)
                t_a = t_d = None
                for c in range(4):
                    for hh in range(2):
                        pb = mmi[0] % 4
                        cx.wait("pe", mm_free[pb])
                        for kc in range(8):
                            mm = nc.tensor.matmul(psum[pb][:, :], lhsT=hT[:, kc, c * 128:(c + 1) * 128], rhs=wkv[:, kc, 1024 + hh * 512:1536 + hh * 512],
                                                  start=(kc == 0), stop=(kc == 7))
                        t_mm = cx.sig("pe", mm)
                        if hh == 0:
                            cx.wait("act", t_mm)
                            t_ev = t_a = cx.sig("act", nc.scalar.copy(out=vmst[b][:, :, c, 0:64], in_=psum[pb][:, :].rearrange("p (h e) -> p h e", h=8)))
                        else:
                            cx.wait("dve", t_mm)
                            t_ev = t_d = cx.sig("dve", nc.vector.tensor_copy(out=vdst[b][:, :, c, 0:128], in_=psum[pb][:, :].rearrange("p (h e) -> p h e", h=4)))
                        mm_free[pb] = t_ev
                        mmi[0] += 1
                pipe.hT_free[b] = t_mm
                cx.wait("sp", t_a, t_d)
                cx.dma("sp", VM_scr[:, :, 4 * T:4 * T + 4, :].rearrange("h p c e -> p h (c e)"), vmst[b][:].rearrange("p h c e -> p h (c e)"), s_vst[b])
                vst_free[b] = cx.dma("sp", VD_scr[:, :, 4 * T:4 * T + 4, :].rearrange("h p c e -> p h (c e)"),
                                     vdst[b][:].rearrange("p h c e -> p h (c e)"), s_vst[b])

            for T in range(3):
                pipe.L(T)
            pipe.Q(0); pipe.H(0); pipe.Q(1); pipe.H(1); R1(0)
            for T in range(NTA):
                pipe.L(T + 3)
                pipe.Q(T + 2)
                pipe.H(T + 2)
                R1(T + 1)
                M1(T)
            cx.wait("dve", t_hs[0])
            cx.sig("dve", nc.vector.tensor_copy(out=hs_sb[:], in_=psum[5][:, 0:256].rearrange("p (k b) -> p k b", k=8)))
            cx.barrier()

        with ExitStack() as p2:
            sb2 = lambda name, shape, dtype: p2.enter_context(nc.sbuf_tensor('p2_' + name, shape, dtype))
            qst = [sb2(f"qst{i}", [128, 8, 512], BF16) for i in range(2)]
            gconst = sb2("gconst", [128, 3, 32, 32], F32)
            hbar = sb2("hbar", [128, 8], F32)
            hc = sb2("hc", [128, 8, 32], F32)
            hc_bf = sb2("hc_bf", [128, 8, 32], BF16)
            bd = sb2("bd", [128, 4, 64], BF16)
            gm = sb2("gm", [128, 256], F32)
            top8 = sb2("top8", [128, 8, 8], F32)
            sel = sb2("sel", [128, 256], F32)
            mrow = [sb2(f"mrow{i}", [128, 256], BF16) for i in range(4)]
            mst = [sb2(f"mst{i}", [128, 2, 512], BF16) for i in range(2)]
            t_w = t_wq
            s_g = cx.sem("ld_gc")
            s_qst = [cx.sem("st_q") for _ in range(2)]
            s_mst = [cx.sem("st_m") for _ in range(2)]
            t_gc = cx.dma("sp", gconst[:], gconst_d[:, :, :, :], s_g)
            x_own_v = x_own.rearrange("(t c p) d -> t p c d", c=4, p=128)
            pipe = Pipe(sb2, x_own_v, NTO)
            for T in range(3):
                pipe.L(T)
            t = cx.sig("dve", nc.vector.tensor_reduce(out=hbar[:], in_=hs_sb[:], axis=AX.X, op=ALU.add))
            cx.wait("dve", t)
            t = cx.sig("dve", nc.vector.tensor_scalar(out=hbar[:], in0=hbar[:], scalar1=-1.0 / 32, scalar2=None, op0=ALU.mult))
            cx.wait("dve", t)
            hbar_b = bass.AP(hbar[:].tensor, hbar[:].offset, [hbar[:].ap[0], [1, 8], [0, 32]])
            t = cx.sig("dve", nc.vector.tensor_tensor(out=hc[:], in0=hs_sb[:], in1=hbar_b, op=ALU.add))
            cx.wait("dve", t)
            t_hc = cx.sig("dve", nc.vector.tensor_scalar(out=hc_bf[:], in0=hc[:], scalar1=1.0 / 256, scalar2=None, op0=ALU.mult))
            t_bd0 = cx.sig("dve", nc.vector.memset(bd[:], 0.0))
            cx.wait("pe", t_hc, t_w)
            t_bd = None
            for jp in range(4):
                for kc in range(8):
                    mm = nc.tensor.matmul(psum[jp][:, 0:32], lhsT=wq[:, kc, 1024 + jp * 128:1024 + (jp + 1) * 128], rhs=hc_bf[:, kc, :],
                                          start=(kc == 0), stop=(kc == 7))
                t_mm = cx.sig("pe", mm)
                cx.wait("dve", t_mm, t_bd0)
                cx.sig("dve", nc.vector.tensor_copy(out=bd[0:64, jp, 0:32], in_=psum[jp][0:64, 0:32]))
                t_bd = cx.sig("dve", nc.vector.tensor_copy(out=bd[64:128, jp, 32:64], in_=psum[jp][64:128, 0:32]))
            mm_free = [t_bd] * 4
            qst_free = [None, None]
            mst_free = [None, None]
            mrow_free = [None] * 4
            mmi = [0]

            t_qs = {}
            t_mrs = {}

            def Qpart(n):
                b = n % 2
                hT = pipe.hT[b]
                cx.wait("pe", *pipe.t_hT[n], t_w)
                cx.wait("act", qst_free[b])
                t_q = []
                for j in range(8):
                    pb = mmi[0] % 4
                    cx.wait("pe", mm_free[pb])
                    for kc in range(8):
                        mm = nc.tensor.matmul(psum[pb][:, :], lhsT=wq[:, kc, j * 128:(j + 1) * 128], rhs=hT[:, kc, :], start=(kc == 0), stop=(kc == 7))
                    t_mm = cx.sig("pe", mm)
                    cx.wait("act", t_mm)
                    t_ev = cx.sig("act", nc.scalar.copy(out=qst[b][:, j, :], in_=psum[pb][:, :]))
                    mm_free[pb] = t_ev
                    t_q.append(t_ev)
                    mmi[0] += 1
                pipe.hT_free[b] = t_mm
                t_qs[n] = t_q
                cx.wait("sp", t_q[-1])
                qst_free[b] = cx.dma("sp", QT_scr[:, :, n * 512:(n + 1) * 512].rearrange("j p t -> p j t"), qst[b][:], s_qst[b])

            def Gfront(n):
                if n < 0:
                    return
                b = n % 2
                t_mr_l = []
                for c in range(4):
                    ci = n * 4 + c
                    cx.wait("pe", t_qs[n][3], t_bd, gate_free[0])
                    gb = 4
                    for jp in range(4):
                        mm = nc.tensor.matmul(psum[gb][:, jp * 64:(jp + 1) * 64], lhsT=qst[b][:, jp, c * 128:(c + 1) * 128], rhs=bd[:, jp, :],
                                              start=True, stop=True)
                    t_g = cx.sig("pe", mm)
                    cx.wait("dve", t_g, t_gc)
                    vb_ap = gconst[:, 0, ci, :]
                    vb_b = bass.AP(vb_ap.tensor, vb_ap.offset, [vb_ap.ap[0], [0, 8], vb_ap.ap[1]])
                    va_ap = gconst[:, 1, ci, :]
                    va_b = bass.AP(va_ap.tensor, va_ap.offset, [va_ap.ap[0], [0, 8], va_ap.ap[1]])
                    ow_ap = gconst[:, 2, ci, :]
                    ow_b = bass.AP(ow_ap.tensor, ow_ap.offset, [ow_ap.ap[0], [0, 8], ow_ap.ap[1]])
                    gm3 = gm[:].rearrange("p (h k) -> p h k", h=8)
                    sel3 = sel[:].rearrange("p (h k) -> p h k", h=8)
                    t = cx.sig("dve", nc.vector.tensor_tensor(out=gm3, in0=psum[gb][:, 0:256].rearrange("p (h k) -> p h k", h=8), in1=vb_b, op=ALU.add))
                    gate_free[0] = t
                    cx.wait("dve", t)
                    for hd in range(8):
                        t = cx.sig("dve", nc.vector.max(out=top8[:, hd, :], in_=gm[:, hd * 32:(hd + 1) * 32]))
                    cx.wait("dve", t)
                    t8 = top8[:, :, 2:3]
                    thr_b = bass.AP(t8.tensor, t8.offset, [t8.ap[0], [8, 8], [0, 32]])
                    t = cx.sig("dve", nc.vector.tensor_tensor(out=sel3, in0=gm3, in1=thr_b, op=ALU.is_ge))
                    cx.wait("dve", t)
                    t = cx.sig("dve", nc.vector.tensor_tensor(out=sel3, in0=sel3, in1=va_b, op=ALU.mult))
                    cx.wait("dve", t)
                    t = cx.sig("dve", nc.vector.tensor_tensor(out=sel3, in0=sel3, in1=ow_b, op=ALU.add))
                    cx.wait("dve", t, mrow_free[c])
                    t_mr_l.append(cx.sig("dve", nc.vector.tensor_scalar(out=mrow[c][:], in0=sel[:], scalar1=-1.0, scalar2=-NEG, op0=ALU.add, op1=ALU.mult)))
                t_mrs[n] = t_mr_l

            def Gback(n):
                if n < 0:
                    return
                b = n % 2
                cx.wait("dve", mst_free[b])
                for c in range(4):
                    g2 = c % 2
                    bk = 7 if g2 else 6
                    cx.wait("pe", t_mrs[n][c], tp_free[g2])
                    tp = tp_views[bk]
                    for hf in range(2):
                        tr = nc.tensor.transpose(tp[:, hf * 128:(hf + 1) * 128], mrow[c][:, hf * 128:(hf + 1) * 128], ident[:])
                    t_tp = cx.sig("pe", tr)
                    mrow_free[c] = t_tp
                    cx.wait("dve", t_tp)
                    t_e = cx.sig("dve", nc.vector.tensor_copy(out=mst[b][:, :, c * 128:(c + 1) * 128], in_=tp[:, 0:256].rearrange("p (f t) -> p f t", f=2)))
                    tp_free[g2] = t_e
                cx.wait("sp", t_e)
                mst_free[b] = cx.dma("sp", QM_scr[:, n * 512:(n + 1) * 512].rearrange("(f p) t -> p f t", p=128), mst[b][:], s_mst[b])

            pipe.Q(0); pipe.H(0); pipe.Q(1); pipe.H(1); pipe.R(0)
            for n in range(NTO):
                pipe.L(n + 3)
                pipe.Q(n + 2)
                Gfront(n - 1)
                pipe.R(n + 1)
                pipe.H(n + 2)
                Qpart(n)
                Gback(n - 1)
            Gfront(NTO - 1)
            Gback(NTO - 1)
            cx.barrier()

        p12.close()
        wz = sb("wz", [128, 8, 1024], BF16)
        wo = sb("wo", [128, 8, 1024], BF16)
        s_w4 = cx.sem("ld_w4")
        for i in range(2):
            cx.dma("pool", wz[:, :, i * 512:(i + 1) * 512], w_in_v[:, :, 3072 + i * 512:3072 + (i + 1) * 512], s_w4)
        for i in range(2):
            t_w4 = cx.dma("pool", wo[:, :, i * 512:(i + 1) * 512], w_out_v[:, :, i * 512:(i + 1) * 512], s_w4)
        with ExitStack() as p3:
            sb3 = lambda name, shape, dtype: p3.enter_context(nc.sbuf_tensor('p3_' + name, shape, dtype))
            NSET = 3
            KA = [sb3(f"KA{i}", [128, S], BF16) for i in range(NSET)]
            QA = [sb3(f"QA{i}", [128, S // 2], BF16) for i in range(NSET)]
            VA = [sb3(f"VA{i}", [128, 64 * 129], BF16) for i in range(NSET)]
            cmask = sb3("cmask", [128, 128], BF16)
            PT = [sb3(f"PT{i}", [128, 512], BF16) for i in range(4)]
            n0 = sb3("n0", [128, 32, 128], F32)
            tmpn = sb3("tmpn", [128, 4, 128], F32)
            stage = [sb3(f"stg{i}", [128, 4, 128], BF16) for i in range(2)]
            rden = sb3("rden", [128, 8], F32)
            s_cm = cx.sem("ld_cm")
            s_grp = [cx.sem("ld_grp") for _ in range(NSET)]
            s_stg = [cx.sem("st_stg") for _ in range(2)]
            t_cm = cx.dma("sp", cmask[:], cmask_d[:, :], s_cm)
            for i in range(NSET):
                t_cm = cx.dma("sp", KA[i][64:100, :], kaug_d[:, :], s_cm)
            grp_free = [None] * NSET
            qmask_state = [None] * NSET
            group_order = [7, 0, 6, 1, 5, 2, 14, 3, 15, 4, 12, 13, 8, 9, 10, 11]
            S_banks = [0, 1, 6, 7]
            S_free = [None] * 4
            PT_free = [None] * 4
            acc_free = [None, None]
            stg_free = [None, None]
            u = 0
            rd = 0
            pend = []
            PV_LAG = 3

            def emit_pv(pv):
                cx.wait("pe", pv["t_act"], acc_free[pv["a"]] if pv["first"] else None)
                for (ks, c_lo, c_hi, col) in pv["pieces"]:
                    for c in range(c_lo, c_hi + 1):
                        bank = pv["banks"][c // 2]
                        off = (c % 2) * 129
                        pc = col + (c - c_lo) * 128
                        mm = nc.tensor.matmul(psum[bank][:, off:off + pv["e1"]], lhsT=PT[pv["pt"]][:, pc:pc + 128],
                                              rhs=VA[pv["set"]][:, ks * pv["e1"]:(ks + 1) * pv["e1"]],
                                              start=pv["starts"][(ks, c)], stop=pv["last"], skip_group_check=True)
                t_pv = cx.sig("pe", mm)
                PT_free[pv["pt"]] = t_pv
                if pv["last"]:
                    pv["finish"](t_pv)
                return t_pv

            for gpos, g in enumerate(group_order):
                st = gpos % NSET
                if g < 8:
                    kj, r0, e, gi_q = g // 2, (g % 2) * 64, 64, g
                    vsrc = VM_scr[g]
                    ocol = g * 64
                    dh, mp = None, None
                else:
                    dh, mp = (g - 8) // 2, (g - 8) % 2
                    kj, r0, e, gi_q = 4 + dh, mp * 64, 128, 8 + dh
                    vsrc = VD_scr[dh]
                    ocol = 512 + dh * 128
                e1 = e + 1
                slope = (MOBA_SLOPES + DIFF_SLOPES)[gi_q]
                W = int(np.ceil((ALIBI_THR / slope + 127.0) / 128.0))
                cx.wait("sp", grp_free[st])
                cx.dma("sp", KA[st][0:64, :], KT_scr[kj, r0:r0 + 64, :], s_grp[st])
                cx.dma("sp", QA[st][0:64, :], QT_scr[kj, r0:r0 + 64, :], s_grp[st])
                cx.dma("sp", QA[st][64:68, :], qaug_d[gi_q, :, :], s_grp[st])
                if g < 8:
                    cx.dma("sp", QA[st][68:100, :], QM_scr[g * 32:(g + 1) * 32, :], s_grp[st])
                    qmask_state[st] = "moba"
                elif qmask_state[st] != "zero":
                    cx.dma("sp", QA[st][68:100, :], zrows_d[:, :], s_grp[st])
                    qmask_state[st] = "zero"
                vv = vsrc.rearrange("p kt e -> p (kt e)")
                for q2 in range(2):
                    t_grp = cx.dma("sp", VA[st][:, q2 * 32 * e1:(q2 + 1) * 32 * e1], vv[:, q2 * 32 * e1:(q2 + 1) * 32 * e1], s_grp[st])
                t_last_pe = None
                for n in range(NTO):
                    a = rd % 2
                    banks = (2, 3) if a == 0 else (4, 5)
                    base = 4 * (2 * n + 1)
                    sus = [dict(pieces=[(base + c, c, c, i * 128) for i, c in enumerate([3, 2, 1, 0])], ncols=512, mask=True)]
                    cur = None
                    for ks in range(base + 2, max(-1, base - W), -1):
                        c_lo = max(0, ks - base + 1)
                        c_hi = min(3, ks - base + W - 1)
                        c = c_lo
                        while c <= c_hi:
                            if cur is None or cur["ncols"] == 512:
                                cur = dict(pieces=[], ncols=0, mask=False)
                                sus.append(cur)
                            take = min((512 - cur["ncols"]) // 128, c_hi - c + 1)
                            cur["pieces"].append((ks, c, c + take - 1, cur["ncols"]))
                            cur["ncols"] += take * 128
                            c += take
                    nun = len(sus)
                    bank_started = [False, False]
                    def make_finish(g=g, n=n, a=a, banks=banks, e=e, e1=e1, dh=dh, mp=mp, ocol=ocol):
                        def fin(t_pv):
                            sg_ = a
                            cx.wait("dve", t_pv, stg_free[sg_])
                            for c in range(4):
                                bank = banks[c // 2]
                                off = (c % 2) * 129
                                t = cx.sig("dve", nc.vector.reciprocal(out=rden[:, a * 4 + c:a * 4 + c + 1], in_=psum[bank][:, off + e:off + e + 1]))
                            cx.wait("dve", t)
                            for c in range(4):
                                bank = banks[c // 2]
                                off = (c % 2) * 129
                                sc = rden[:, a * 4 + c:a * 4 + c + 1]
                                src = psum[bank][:, off:off + e]
                                if g < 8:
                                    t = cx.sig("dve", nc.vector.tensor_scalar(out=stage[sg_][:, c, 0:64], in0=src, scalar1=sc, scalar2=None, op0=ALU.mult))
                                elif mp == 0:
                                    t = cx.sig("dve", nc.vector.tensor_scalar(out=n0[:, n * 4 + c, :], in0=src, scalar1=sc, scalar2=None, op0=ALU.mult))
                                else:
                                    t = cx.sig("dve", nc.vector.tensor_scalar(out=tmpn[:, c, :], in0=src, scalar1=sc, scalar2=None, op0=ALU.mult))
                            acc_free[a] = t
                            if g >= 8 and mp == 1:
                                cx.wait("dve", t)
                                for c in range(4):
                                    t = cx.sig("dve", nc.vector.scalar_tensor_tensor(out=stage[sg_][:, c, :], in0=tmpn[:, c, :], scalar=neglam[:, 0:1],
                                                                                     in1=n0[:, n * 4 + c, :], op0=ALU.mult, op1=ALU.add))
                            if g < 8 or mp == 1:
                                cx.wait("pool", t)
                                stg_free[sg_] = cx.dma("pool", att_scr[n * 512:(n + 1) * 512, ocol:ocol + e].rearrange("(c p) f -> p c f", p=128),
                                                       stage[sg_][:, :, 0:e], s_stg[sg_])
                        return fin
                    fin = make_finish()
                    for uu in range(nun):
                        su = sus[uu]
                        ncols = su["ncols"]
                        si = u % 4
                        sbk = S_banks[si]
                        cx.wait("pe", S_free[si], t_grp, t_cm)
                        for (ks, c_lo, c_hi, col) in su["pieces"]:
                            wcol = (c_hi - c_lo + 1) * 128
                            mmq = nc.tensor.matmul(psum[sbk][:, col:col + wcol], lhsT=KA[st][0:KROWS, ks * 128:(ks + 1) * 128],
                                                   rhs=QA[st][0:KROWS, n * 512 + c_lo * 128:n * 512 + (c_hi + 1) * 128], start=True, stop=True)
                        t_s = cx.sig("pe", mmq)
                        if su["mask"]:
                            cx.wait("dve", t_s, t_cm)
                            pv3 = psum[sbk][:, :].rearrange("p (b q) -> p b q", b=4)
                            cm_ap = cmask[:, :]
                            cm_b = bass.AP(cm_ap.tensor, cm_ap.offset, [cm_ap.ap[0], [0, 4], cm_ap.ap[1]])
                            t_s = cx.sig("dve", nc.vector.tensor_tensor(out=pv3, in0=pv3, in1=cm_b, op=ALU.add))
                        pti = u % 4
                        cx.wait("act", t_s, PT_free[pti])
                        t_act = cx.sig("act", nc.scalar.activation(out=PT[pti][:, 0:ncols], in_=psum[sbk][:, 0:ncols], func=AF.Exp, scale=0.125))
                        S_free[si] = t_act
                        starts = {}
                        for (ks, c_lo, c_hi, col) in su["pieces"]:
                            for c in range(c_lo, c_hi + 1):
                                starts[(ks, c)] = not bank_started[c // 2]
                                bank_started[c // 2] = True
                        pend.append(dict(t_act=t_act, a=a, first=(uu == 0), banks=banks, e1=e1, pt=pti, set=st, pieces=su["pieces"],
                                         starts=starts, last=(uu == nun - 1), finish=fin))
                        if len(pend) > PV_LAG:
                            t_last_pe = emit_pv(pend.pop(0))
                        u += 1
                    rd += 1
                while pend:
                    t_last_pe = emit_pv(pend.pop(0))
                grp_free[st] = t_last_pe
            cx.barrier()

        with ExitStack() as p4:
            sb4 = lambda name, shape, dtype: p4.enter_context(nc.sbuf_tensor('p4_' + name, shape, dtype))
            xr = [sb4(f"xr{i}", [128, 4, D], F32) for i in range(2)]
            abuf = [sb4(f"ab{i}", [128, 4, D], BF16) for i in range(2)]
            zs = [sb4(f"zs{i}", [128, 512], F32) for i in range(2)]
            ybuf = sb4("ybuf", [128, 4, D], BF16)
            yT = [sb4(f"yT{i}", [128, 8, 512], BF16) for i in range(2)]
            tmp2 = [sb4(f"tmpo{i}", [128, 512], F32) for i in range(2)]
            ssd = sb4("ssd", [128, 32, 4], F32)
            rsd = sb4("rsd", [128, 32, 4], F32)
            ssf = sb4("ssf", [128, 32], F32)
            rsf = sb4("rsf", [128, 32], F32)
            mh4 = sb4("mh4", [128, 4], F32)
            t_w = t_w4
            s_xr = [cx.sem("ld_xr") for _ in range(2)]
            s_a = [cx.sem("ld_a4") for _ in range(2)]
            s_o = [cx.sem("st_o4") for _ in range(2)]
            t_mh = cx.sig("pool", nc.gpsimd.memset(mh4[:], -0.5))
            cx.wait("dve", t_w)
            gate_k = bass.AP(gate_bc.tensor, gate_bc.offset, [gate_bc.ap[0], [0, 8], gate_bc.ap[1]])
            t_wo = cx.sig("dve", nc.vector.tensor_tensor(out=wo[:], in0=wo[:], in1=gate_k, op=ALU.mult))
            x_own_v = x_own.rearrange("(t c p) d -> t p c d", c=4, p=128)
            att_v = att_scr.rearrange("(t c p) d -> t p c d", c=4, p=128)
            out_v = out_d.rearrange("(t c p) d -> t p c d", c=4, p=128)
            pipe = Pipe(sb4, x_own_v, NTO, nx=2)
            xr_free = [None, None]
            a_free = [None, None]
            t_xr, t_ab = {}, {}
            mm_free = [None] * 4
            zs_free = [None, None]
            t2_free = [None, None]
            y_free = [None]
            yT_free = [None, None]
            t_yTs = {}
            t_ys = {}
            ytp_free = [None, None]
            mmi = [0]
            zi = [0]

            def LAa(n):
                if n >= NTO:
                    return
                b = n % 2
                cx.wait("sp", a_free[b])
                t_ab[n] = cx.dma("sp", abuf[b][:], att_v[n], s_a[b])

            def LAx(n):
                if n >= NTO:
                    return
                b = n % 2
                cx.wait("sp", xr_free[b])
                t_xr[n] = cx.dma("sp", xr[b][:], x_own_v[n], s_xr[b])

            def A4(n):
                if n >= NTO:
                    return
                b = n % 2
                hT = pipe.hT[b]
                t_a = t_ab[n]
                t_dn = []
                for c in range(4):
                    ci = n * 4 + c
                    cx.wait("act", t_a)
                    for dh in range(4):
                        cx.wait("act", junk_tok[0])
                        t = cx.sig("act", nc.scalar.activation(out=junk[:, 0:128], in_=abuf[b][:, c, 512 + dh * 128:512 + (dh + 1) * 128], func=AF.Square,
                                                               accum_out=ssd[:, ci, dh:dh + 1]))
                        junk_tok[0] = t
                    cx.wait("pool", t, t_mh)
                    t = cx.sig("pool", nc.gpsimd.tensor_scalar(out=rsd[:, ci, :], in0=ssd[:, ci, :], scalar1=1.0 / 128, scalar2=EPS, op0=ALU.mult, op1=ALU.add))
                    cx.wait("pool", t)
                    t = cx.sig("pool", nc.gpsimd.tensor_tensor(out=rsd[:, ci, :], in0=rsd[:, ci, :], in1=mh4[:], op=ALU.pow))
                    cx.wait("dve", t, t_a)
                    for dh in range(4):
                        sl = abuf[b][:, c, 512 + dh * 128:512 + (dh + 1) * 128]
                        t = cx.sig("dve", nc.vector.scalar_tensor_tensor(out=sl, in0=sl, scalar=rsd[:, ci, dh:dh + 1], in1=subg_bc[:], op0=ALU.mult, op1=ALU.mult))
                    t_dn.append(t)
                cx.wait("pe", *pipe.t_hT[n], t_w)
                t_y = []
                for c in range(4):
                    for hh in range(2):
                        pb = mmi[0] % 4
                        cx.wait("pe", mm_free[pb])
                        for kc in range(8):
                            mm = nc.tensor.matmul(psum[pb][:, :], lhsT=hT[:, kc, c * 128:(c + 1) * 128], rhs=wz[:, kc, hh * 512:(hh + 1) * 512],
                                                  start=(kc == 0), stop=(kc == 7))
                        t_mm = cx.sig("pe", mm)
                        zb = zi[0] % 2
                        cx.wait("act", t_mm, zs_free[zb])
                        t_z = cx.sig("act", nc.scalar.activation(out=zs[zb][:], in_=psum[pb][:, :], func=AF.Silu))
                        mm_free[pb] = t_z
                        cx.wait("dve", t_z, t_a, t_dn[c], y_free[0])
                        t = cx.sig("dve", nc.vector.tensor_tensor(out=ybuf[:, c, hh * 512:(hh + 1) * 512], in0=abuf[b][:, c, hh * 512:(hh + 1) * 512], in1=zs[zb][:], op=ALU.mult))
                        zs_free[zb] = t
                        mmi[0] += 1
                        zi[0] += 1
                    t_y.append(t)
                pipe.hT_free[b] = t_mm
                a_free[b] = t_y[-1]
                t_ys[n] = t_y

            def A4b(n):
                if n >= NTO:
                    return
                b = n % 2
                t_y = t_ys[n]
                t_yT = []
                t_tps = []
                for c in range(4):
                    bk = (4, 5, 6, 7)[c]
                    cx.wait("pe", t_y[c], ytp_free[c] if c < 2 else tp_free[c - 2])
                    tp = tp_views[bk]
                    for kc in range(8):
                        tr = nc.tensor.transpose(tp[:, kc * 128:(kc + 1) * 128], ybuf[:, c, kc * 128:(kc + 1) * 128], ident[:])
                    t_tps.append(cx.sig("pe", tr))
                t_tp = t_tps[-1]
                for c in range(4):
                    bk = (4, 5, 6, 7)[c]
                    tp = tp_views[bk]
                    cx.wait("act", t_tps[c], yT_free[b])
                    t_e = cx.sig("act", nc.scalar.copy(out=yT[b][:, :, c * 128:(c + 1) * 128], in_=tp[:, :].rearrange("p (k t) -> p k t", k=8)))
                    if c < 2:
                        ytp_free[c] = t_e
                    else:
                        tp_free[c - 2] = t_e
                    t_yT.append(t_e)
                y_free[0] = t_tp
                t_yTs[n] = t_yT

            def B4(n):
                b = n % 2
                t_yT = t_yTs[n]
                for c in range(4):
                    ci = n * 4 + c
                    for hh in range(2):
                        pb = mmi[0] % 4
                        cx.wait("pe", mm_free[pb], t_yT[c], t_wo)
                        for kc in range(8):
                            mm = nc.tensor.matmul(psum[pb][:, :], lhsT=yT[b][:, kc, c * 128:(c + 1) * 128], rhs=wo[:, kc, hh * 512:(hh + 1) * 512],
                                                  start=(kc == 0), stop=(kc == 7))
                        t_mm = cx.sig("pe", mm)
                        xs = xr[b][:, c, hh * 512:(hh + 1) * 512]
                        cx.wait("dve", t_mm, t_xr[n])
                        t = cx.sig("dve", nc.vector.tensor_tensor(out=xs, in0=psum[pb][:, :], in1=xs, op=ALU.add))
                        mm_free[pb] = t
                        mmi[0] += 1
                        zi[0] += 1
                    cx.wait("act", t)
                    cx.wait("act", junk_tok[0])
                    t = cx.sig("act", nc.scalar.activation(out=junk[:], in_=xr[b][:, c, :], func=AF.Square, accum_out=ssf[:, ci:ci + 1]))
                    junk_tok[0] = t
                    cx.wait("pool", t)
                    t = cx.sig("pool", nc.gpsimd.tensor_scalar(out=rsf[:, ci:ci + 1], in0=ssf[:, ci:ci + 1], scalar1=1.0 / D, scalar2=EPS, op0=ALU.mult, op1=ALU.add))
                    cx.wait("pool", t)
                    t = cx.sig("pool", nc.gpsimd.tensor_tensor(out=rsf[:, ci:ci + 1], in0=rsf[:, ci:ci + 1], in1=mhalf[:], op=ALU.pow))
                    cx.wait("dve", t, t_fing)
                    t = cx.sig("dve", nc.vector.scalar_tensor_tensor(out=xr[b][:, c, :], in0=xr[b][:, c, :], scalar=rsf[:, ci:ci + 1], in1=fing_bc[:],
                                                                     op0=ALU.mult, op1=ALU.mult))
                yT_free[b] = t_mm
                cx.wait("act", t)
                xr_free[b] = cx.dma("act", out_v[n], xr[b][:], s_o[b])

            for T in range(2):
                pipe.L(T)
            LAa(0)
            LAa(1)
            LAx(0)
            LAx(1)
            pipe.Q(0); pipe.H(0); pipe.Q(1); pipe.H(1); pipe.R(0)
            A4(0)
            A4b(0)
            LAa(2)
            for n in range(NTO):
                pipe.L(n + 2)
                pipe.Q(n + 2)
                pipe.H(n + 2)
                pipe.R(n + 1)
                LAx(n + 1) if n >= 1 else None
                A4(n + 1)
                LAa(n + 3)
                B4(n)
                A4b(n + 1)
            fin_toks = cx.final_dma_toks()
            cx.wait("pool", *fin_toks)
            cx.wait("sp", *fin_toks)
        return nc, cx


def _own_pos(j):
    return np.concatenate([np.arange((2 * n + j) * 512, (2 * n + j + 1) * 512) for n in range(NTO)])


def _consts(j):
    pos = _own_pos(j)
    a_q = (pos // 256).astype(np.float32)
    b_q = (pos % 256).astype(np.float32)
    qaug = np.zeros((12, 4, S // 2), np.float32)
    for i, m in enumerate(MOBA_SLOPES + DIFF_SLOPES):
        qaug[i, 0] = -8.0 * m * 256.0 * a_q
        qaug[i, 1] = -8.0 * m * b_q
        qaug[i, 2] = 8.0 * m
        qaug[i, 3] = 8.0 * m
    lp = np.arange(S)
    gp = lp - 512 * (1 - j)
    real = gp >= 0
    gpc = np.maximum(gp, 0)
    kaug = np.zeros((36, S), np.float32)
    kaug[0] = 1.0
    kaug[1] = 1.0
    kaug[2] = np.where(real, 256.0 * (gpc // 256), -float(2 ** 22))
    kaug[3] = np.where(real, gpc % 256, 0.0)
    kaug[4 + lp[real] // 256, lp[real]] = 1.0
    kk = np.arange(128)[:, None]
    qq = np.arange(128)[None, :]
    cm = np.where(kk > qq, NEG, 0.0).astype(np.float32)
    gc = np.zeros((128, 3, 32, 32), np.float32)
    lb = np.arange(32)
    gb = lb - 2 * (1 - j)
    for ci in range(32):
        cur = pos[ci * 128] // 256
        valid = (gb >= 0) & (gb < cur)
        gc[:, 0, ci, :] = np.where(valid, 0.0, -1e30)[None]
        gc[:, 1, ci, :] = valid.astype(np.float32)[None]
        gc[:, 2, ci, :] = (gb == cur).astype(np.float32)[None]
    return dict(qaug=qaug.astype(BF), kaug=kaug.astype(BF), cmask=cm.astype(BF), gconst=gc,
                ident=np.eye(128, dtype=np.float32).astype(BF), zrows=np.zeros((32, S // 2), BF))


def make_in_maps(inputs):
    f = lambda k: np.ascontiguousarray(np.asarray(inputs[k], dtype=np.float32))
    x = f("x")
    c = f("c")
    shared = dict(
        ln_g=f("ln_g")[0][None, :], b_ada=f("b_ada")[0][None, :], final_g=f("final_g")[None, :], subln_g=f("subln_g")[0][None, :],
        lambda_q1=f("lambda_q1")[0][None, :], lambda_k1=f("lambda_k1")[0][None, :], lambda_q2=f("lambda_q2")[0][None, :],
        lambda_k2=f("lambda_k2")[0][None, :], w_ada=f("w_ada")[0], w_in=f("w_in")[0], w_out=f("w_out")[0])
    consts = [_consts(0), _consts(1)]
    in_maps = []
    for core in range(8):
        b, j = core // 2, core % 2
        m = dict(shared)
        m.update(consts[j])
        if j == 1:
            m["x_all"] = x[b]
        else:
            m["x_all"] = np.concatenate([np.zeros((512, D), np.float32), x[b][:S - 512]], axis=0)
        m["x_own"] = np.ascontiguousarray(x[b][_own_pos(j)])
        m["c_t"] = np.ascontiguousarray(c[b].reshape(8, 128).T)
        in_maps.append(m)
    return in_maps


def kernel(**inputs):
    nc, _ = build_program()
    in_maps = make_in_maps(inputs)
    res = run_bass_kernel_spmd(nc, in_maps, core_ids=list(range(8)))
    out = np.empty((4, S, D), np.float32)
    for core in range(8):
        b, j = core // 2, core % 2
        out[b, _own_pos(j)] = res.results[core]["out"]
    return out
```

```python
import numpy as np
import ml_dtypes
from contextlib import ExitStack
import concourse.bass as bass
import concourse.mybir as mybir
from concourse.alu_op_type import AluOpType as ALU
from concourse.bass_utils import run_bass_kernel_spmd

F32 = mybir.dt.float32
BF16 = mybir.dt.bfloat16
AF = mybir.ActivationFunctionType
AX = mybir.AxisListType
BF = ml_dtypes.bfloat16

S = 8192
D = 1024
NTA = 16
NTO = 8
EPS = 1e-6
NEG = -float(2 ** 18)
KROWS = 100
MOBA_SLOPES = [2.0 ** (-8.0 * (i + 1) / 8) for i in range(8)]
DIFF_SLOPES = [2.0 ** (-8.0 * (i + 1) / 4) for i in range(4)]
LAM_INIT = 0.8 - 0.6 * 1.0
ALIBI_THR = 40.0
DEBUG = False
gate_free = [None]
junk_tok = [None]


class Sem:
    def __init__(self, nc, es, name):
        self.h = es.enter_context(nc.semaphore(name))
        self.n = 0
        self.name = name


class Ctx:
    def __init__(self, nc, es):
        self.nc = nc
        self.es = es
        self.waited = {}
        self.nsem = 0
        self.E = {"pe": nc.tensor, "act": nc.scalar, "dve": nc.vector, "pool": nc.gpsimd, "sp": nc.sync}
        self.prog = {}
        self.last = {}
        self.dma_toks = []
        self.bar = self.sem("bar")
        self.new_prog()

    def sem(self, name):
        self.nsem += 1
        return Sem(self.nc, self.es, f"{name}_{self.nsem}")

    def new_prog(self):
        for k in ["pe", "act", "dve", "pool"]:
            self.prog[k] = self.sem(k + "_prog")

    def wait(self, eng, *toks):
        for t in toks:
            if t is None:
                continue
            if isinstance(t, list):
                self.wait(eng, *t)
                continue
            sem, val = t
            key = (eng, sem.name)
            if self.waited.get(key, 0) >= val:
                continue
            self.E[eng].wait_ge(sem.h, val)
            self.waited[key] = val

    def sig(self, eng, inst):
        sem = self.prog[eng]
        inst.then_inc(sem.h, 1)
        sem.n += 1
        tok = (sem, sem.n)
        self.last[eng] = tok
        return tok

    def dma(self, eng, out, in_, sem, **kw):
        inst = self.E[eng].dma_start(out=out, in_=in_, **kw)
        inst.then_inc(sem.h, 16)
        sem.n += 16
        tok = (sem, sem.n)
        self.dma_toks.append(tok)
        return tok

    def final_dma_toks(self):
        best = {}
        for sem, val in self.dma_toks:
            if sem.name not in best or best[sem.name][1] < val:
                best[sem.name] = (sem, val)
        return list(best.values())

    def barrier(self):
        toks = list(self.last.values()) + self.final_dma_toks()
        self.wait("sp", *toks)
        self.nc.sync.sem_inc(self.bar.h, 1)
        self.bar.n += 1
        for e in ["pe", "act", "dve", "pool"]:
            self.E[e].wait_ge(self.bar.h, self.bar.n)
        self.dma_toks = []
        self.last = {}
        self.new_prog()


def bcast_rows(handle, n):
    return bass.AP(handle, 0, [[0, 128], [1, n]])


def build_program():
    gate_free[0] = None
    junk_tok[0] = None
    nc = bass.Bass("TRN2", target_bir_lowering=False)
    dt = nc.dram_tensor
    x_all = dt("x_all", [S, D], F32, kind="ExternalInput").ap()
    x_own = dt("x_own", [S // 2, D], F32, kind="ExternalInput").ap()
    c_t = dt("c_t", [128, 8], F32, kind="ExternalInput").ap()
    ln_g = dt("ln_g", [1, D], F32, kind="ExternalInput")
    b_ada = dt("b_ada", [1, 3 * D], F32, kind="ExternalInput")
    final_g = dt("final_g", [1, D], F32, kind="ExternalInput")
    subln_g = dt("subln_g", [1, 128], F32, kind="ExternalInput")
    lams = [dt(n, [1, 64], F32, kind="ExternalInput") for n in ("lambda_q1", "lambda_k1", "lambda_q2", "lambda_k2")]
    w_ada = dt("w_ada", [D, 3 * D], F32, kind="ExternalInput").ap()
    w_in = dt("w_in", [D, 4 * D], F32, kind="ExternalInput").ap()
    w_out = dt("w_out", [D, D], F32, kind="ExternalInput").ap()
    ident_d = dt("ident", [128, 128], BF16, kind="ExternalInput").ap()
    kaug_d = dt("kaug", [36, S], BF16, kind="ExternalInput").ap()
    qaug_d = dt("qaug", [12, 4, S // 2], BF16, kind="ExternalInput").ap()
    cmask_d = dt("cmask", [128, 128], BF16, kind="ExternalInput").ap()
    gconst_d = dt("gconst", [128, 3, 32, 32], F32, kind="ExternalInput").ap()
    zrows_d = dt("zrows", [32, S // 2], BF16, kind="ExternalInput").ap()
    out_d = dt("out", [S // 2, D], F32, kind="ExternalOutput").ap()
    skind = "ExternalOutput" if DEBUG else "Internal"
    KT_scr = dt("KT_scr", [8, 128, S], BF16, kind=skind).ap()
    VM_scr = dt("VM_scr", [8, 128, 64, 65], BF16, kind=skind).ap()
    VD_scr = dt("VD_scr", [4, 128, 64, 129], BF16, kind=skind).ap()
    QT_scr = dt("QT_scr", [8, 128, S // 2], BF16, kind=skind).ap()
    QM_scr = dt("QM_scr", [256, S // 2], BF16, kind=skind).ap()
    att_scr = dt("att_scr", [S // 2, D], BF16, kind=skind).ap()

    w_in_v = w_in.rearrange("(kc p) c -> p kc c", p=128)
    w_ada_v = w_ada.rearrange("(kc p) c -> p kc c", p=128)
    w_out_v = w_out.rearrange("(kc p) c -> p kc c", p=128)

    with ExitStack() as es:
        cx = Ctx(nc, es)
        sb = lambda name, shape, dtype: es.enter_context(nc.sbuf_tensor(name, shape, dtype))
        psum = [es.enter_context(nc.psum_tensor(f"ps{i}", [128, 512], F32)) for i in range(8)]
        ident = sb("ident_sb", [128, 128], BF16)
        mod_bc = sb("mod_bc", [128, 3 * D], F32)
        fing_bc = sb("fing_bc", [128, D], F32)
        subg_bc = sb("subg_bc", [128, 128], F32)
        neglam = sb("neglam", [128, 1], F32)
        mhalf = sb("mhalf", [128, 1], F32)
        ones_bf = sb("ones_bf", [128, 1], BF16)
        junk = sb("junk", [128, D], BF16)
        hs_sb = sb("hs_sb", [128, 8, 32], F32)
        shift_bc = mod_bc[:, 0:D]
        geff_bc = mod_bc[:, D:2 * D]
        gate_bc = mod_bc[:, 2 * D:3 * D]
        s_c = cx.sem("ld_const")

        t_ident = cx.dma("sp", ident[:], ident_d[:, :], s_c)
        t_fing = cx.dma("sp", fing_bc[:], bcast_rows(final_g, D), cx.sem("ld_fing"))
        t_subg = cx.dma("sp", subg_bc[:], bcast_rows(subln_g, 128), cx.sem("ld_subg"))
        cx.sig("pool", nc.gpsimd.memset(mhalf[:], -0.5))
        t_ones = cx.sig("pool", nc.gpsimd.memset(ones_bf[:], 1.0))

        p12 = ExitStack()
        wkv = p12.enter_context(nc.sbuf_tensor("wkv_pre", [128, 8, 2048], BF16))
        wq = p12.enter_context(nc.sbuf_tensor("wq_pre", [128, 8, 1536], BF16))
        s_wkv = cx.sem("ld_wkv")
        s_wq = cx.sem("ld_wq")
        for i, c0 in enumerate([512, 2048, 1024, 2560]):
            t_wkv = cx.dma("pool", wkv[:, :, i * 512:(i + 1) * 512], w_in_v[:, :, c0:c0 + 512], s_wkv)
        for i, c0 in enumerate([0, 1536, 512]):
            t_wq = cx.dma("pool", wq[:, :, i * 512:(i + 1) * 512], w_in_v[:, :, c0:c0 + 512], s_wq)
        with ExitStack() as p0:
            sb0 = lambda name, shape, dtype: p0.enter_context(nc.sbuf_tensor('p0_' + name, shape, dtype))
            ct = sb0("ct", [128, 8], F32)
            cact = sb0("cact", [128, 8], F32)
            sg = sb0("sg", [128, 8], F32)
            ones_f = sb0("ones_f", [128, 128], F32)
            crep = sb0("crep", [128, 8, 128], F32)
            bada_bc = sb0("bada_bc", [128, 3 * D], F32)
            lng_bc = sb0("lng_bc", [128, D], F32)
            wa = [sb0(f"wa{i}", [128, 8, 512], F32) for i in range(6)]
            lam_t = [sb0(f"lam{i}", [128, 64], F32) for i in range(4)]
            lprod = sb0("lprod", [128, 2, 64], F32)
            lsum = sb0("lsum", [128, 2], F32)
            lexp = sb0("lexp", [128, 2], F32)
            s_p0 = cx.sem("ld_p0")
            s_wa = [cx.sem("ld_wa") for _ in range(6)]
            t_ct = cx.dma("sp", ct[:], c_t[:, :], s_p0)
            t_bada = cx.dma("sp", bada_bc[:], bcast_rows(b_ada, 3 * D), s_p0)
            t_lng = cx.dma("sp", lng_bc[:], bcast_rows(ln_g, D), s_p0)
            for i in range(4):
                t_lam = cx.dma("sp", lam_t[i][:], bcast_rows(lams[i], 64), s_p0)
            t_p0 = t_lam
            cx.wait("act", t_p0)
            t = cx.sig("act", nc.scalar.activation(out=sg[:], in_=ct[:], func=AF.Sigmoid))
            t_of = cx.sig("dve", nc.vector.memset(ones_f[:], 1.0))
            cx.wait("dve", t, t_p0, t_of)
            t = cx.sig("dve", nc.vector.tensor_tensor(out=cact[:], in0=ct[:], in1=sg[:], op=ALU.mult))
            cx.wait("dve", t)
            for kc in range(8):
                t_crep = cx.sig("dve", nc.vector.tensor_scalar(out=crep[:, kc, :], in0=ones_f[:], scalar1=cact[:, kc:kc + 1],
                                                               scalar2=None, op0=ALU.mult))
            ps_free = [None, None]
            t_was = [cx.dma("sp", wa[cg][:], w_ada_v[:, :, cg * 512:(cg + 1) * 512], s_wa[cg]) for cg in range(6)]
            for cg in range(6):
                b = cg % 2
                cx.wait("pe", t_was[cg], t_crep, ps_free[b])
                for kc in range(8):
                    mm = nc.tensor.matmul(psum[b][:, :], lhsT=crep[:, kc, :], rhs=wa[cg][:, kc, :], start=(kc == 0), stop=(kc == 7))
                t_mm = cx.sig("pe", mm)
                cx.wait("dve", t_mm, t_p0)
                t_ev = cx.sig("dve", nc.vector.tensor_tensor(out=mod_bc[:, cg * 512:(cg + 1) * 512], in0=psum[b][:, :],
                                                             in1=bada_bc[:, cg * 512:(cg + 1) * 512], op=ALU.add))
                ps_free[b] = t_ev
            cx.wait("dve", t_ev)
            t = cx.sig("dve", nc.vector.scalar_tensor_tensor(out=geff_bc, in0=geff_bc, scalar=1.0, in1=lng_bc[:], op0=ALU.add, op1=ALU.mult))
            t1 = cx.sig("dve", nc.vector.tensor_tensor(out=lprod[:, 0, :], in0=lam_t[0][:], in1=lam_t[1][:], op=ALU.mult))
            t2 = cx.sig("dve", nc.vector.tensor_tensor(out=lprod[:, 1, :], in0=lam_t[2][:], in1=lam_t[3][:], op=ALU.mult))
            cx.wait("dve", t2)
            t = cx.sig("dve", nc.vector.tensor_reduce(out=lsum[:], in_=lprod[:], axis=AX.X, op=ALU.add))
            cx.wait("act", t)
            t = cx.sig("act", nc.scalar.activation(out=lexp[:], in_=lsum[:], func=AF.Exp))
            cx.wait("dve", t)
            t = cx.sig("dve", nc.vector.tensor_tensor(out=neglam[:], in0=lexp[:, 1:2], in1=lexp[:, 0:1], op=ALU.subtract))
            cx.wait("dve", t)
            t = cx.sig("dve", nc.vector.tensor_scalar(out=neglam[:], in0=neglam[:], scalar1=-LAM_INIT, scalar2=None, op0=ALU.add))
            cx.wait("dve", t_subg)
            t = cx.sig("dve", nc.vector.tensor_scalar(out=subg_bc[:], in0=subg_bc[:], scalar1=(1.0 - LAM_INIT), scalar2=None, op0=ALU.mult))
            cx.barrier()

        tp_views = {}
        for bk in (4, 5, 6, 7):
            tp_views[bk] = psum[bk][:, :].bitcast(BF16)
        tp_free = {0: None, 1: None}

        class Pipe:
            def __init__(self, alloc, x_v, ntiles, nx=3):
                self.x_v = x_v
                self.nt = ntiles
                self.nx = nx
                self.xb = [alloc(f"xb{i}", [128, 4, D], F32) for i in range(nx)]
                self.hb = [alloc(f"hb{i}", [128, 4, D], BF16) for i in range(2)]
                self.hT = [alloc(f"hT{i}", [128, 8, 512], BF16) for i in range(2)]
                self.tmp = [alloc(f"tmp{i}", [128, D], F32) for i in range(2)]
                self.ssq = alloc("ssq", [128, 4 * ntiles], F32)
                self.rstd = alloc("rstd", [128, 4 * ntiles], F32)
                self.s_x = [cx.sem("ld_x") for _ in range(nx)]
                self.x_free = [None] * nx
                self.hb_free = [None] * 2
                self.hT_free = [None] * 2
                self.t_x, self.t_r, self.t_h, self.t_hT = {}, {}, {}, {}

            def L(self, T):
                if T >= self.nt:
                    return
                b = T % self.nx
                cx.wait("sp", self.x_free[b])
                self.t_x[T] = cx.dma("sp", self.xb[b][:], self.x_v[T], self.s_x[b])

            def Q(self, T):
                if T >= self.nt:
                    return
                xb = self.xb[T % self.nx]
                toks = []
                cx.wait("act", self.t_x[T])
                for c in range(4):
                    col = T * 4 + c
                    cx.wait("act", junk_tok[0])
                    t_ss = cx.sig("act", nc.scalar.activation(out=junk[:], in_=xb[:, c, :], func=AF.Square, accum_out=self.ssq[:, col:col + 1]))
                    junk_tok[0] = t_ss
                    cx.wait("pool", t_ss)
                    t_v = cx.sig("pool", nc.gpsimd.tensor_scalar(out=self.rstd[:, col:col + 1], in0=self.ssq[:, col:col + 1], scalar1=1.0 / D, scalar2=EPS,
                                                                 op0=ALU.mult, op1=ALU.add))
                    cx.wait("pool", t_v)
                    toks.append(cx.sig("pool", nc.gpsimd.tensor_tensor(out=self.rstd[:, col:col + 1], in0=self.rstd[:, col:col + 1], in1=mhalf[:], op=ALU.pow)))
                self.t_r[T] = toks

            def H(self, T):
                if T >= self.nt:
                    return
                xb = self.xb[T % self.nx]
                hb = self.hb[T % 2]
                toks = []
                cx.wait("dve", self.t_x[T], self.hb_free[T % 2])
                for c in range(4):
                    col = T * 4 + c
                    cx.wait("dve", self.t_r[T][c])
                    t_a = cx.sig("dve", nc.vector.scalar_tensor_tensor(out=self.tmp[c % 2][:], in0=xb[:, c, :], scalar=self.rstd[:, col:col + 1], in1=geff_bc,
                                                                       op0=ALU.mult, op1=ALU.mult))
                    cx.wait("dve", t_a)
                    toks.append(cx.sig("dve", nc.vector.tensor_tensor(out=hb[:, c, :], in0=self.tmp[c % 2][:], in1=shift_bc, op=ALU.add)))
                self.t_h[T] = toks
                self.x_free[T % self.nx] = toks[-1]

            def R(self, T):
                if T >= self.nt:
                    return
                hb = self.hb[T % 2]
                hT = self.hT[T % 2]
                toks = []
                for c in range(4):
                    bk = (6, 7)[c % 2]
                    tp = tp_views[bk]
                    cx.wait("pe", self.t_h[T][c], tp_free[c % 2], t_ident)
                    for kc in range(8):
                        tr = nc.tensor.transpose(tp[:, kc * 128:(kc + 1) * 128], hb[:, c, kc * 128:(kc + 1) * 128], ident[:])
                    t_tp = cx.sig("pe", tr)
                    cx.wait("act", t_tp, self.hT_free[T % 2])
                    t_e = cx.sig("act", nc.scalar.copy(out=hT[:, :, c * 128:(c + 1) * 128], in_=tp[:, :].rearrange("p (k t) -> p k t", k=8)))
                    tp_free[c % 2] = t_e
                    toks.append(t_e)
                self.t_hT[T] = toks
                self.hb_free[T % 2] = t_tp

        with ExitStack() as p1:
            sb1 = lambda name, shape, dtype: p1.enter_context(nc.sbuf_tensor('p1_' + name, shape, dtype))
            kst = [sb1(f"kst{i}", [128, 8, 512], BF16) for i in range(2)]
            vmst = [sb1(f"vmst{i}", [128, 8, 4, 65], BF16) for i in range(2)]
            vdst = [sb1(f"vdst{i}", [128, 4, 4, 129], BF16) for i in range(2)]
            s_kst = [cx.sem("st_k") for _ in range(2)]
            s_vst = [cx.sem("st_v") for _ in range(2)]
            t_w = t_wkv
            pre = []
            for i in range(2):
                t1 = cx.sig("dve", nc.vector.memset(vmst[i][:], 1.0))
                t2 = cx.sig("dve", nc.vector.memset(vdst[i][:], 1.0))
                pre = [t1, t2]
            x_all_v = x_all.rearrange("(t c p) d -> t p c d", c=4, p=128)
            pipe = Pipe(sb1, x_all_v, NTA)
            mm_free = [None] * 4
            kst_free = [None, None]
            vst_free = [None, None]
            mmi = [0]
            t_hs = [None]

            def R1(T):
                if T >= NTA:
                    return
                pipe.R(T)
                hb = pipe.hb[T % 2]
                cx.wait("pe", *pipe.t_h[T], t_ones)
                for blk in range(2):
                    for kc in range(8):
                        col = kc * 32 + 2 * T + blk
                        nc.tensor.matmul(psum[5][:, col:col + 1], lhsT=hb[:, 2 * blk, kc * 128:(kc + 1) * 128], rhs=ones_bf[:, 0:1],
                                         start=True, stop=False, skip_group_check=True)
                        mm = nc.tensor.matmul(psum[5][:, col:col + 1], lhsT=hb[:, 2 * blk + 1, kc * 128:(kc + 1) * 128], rhs=ones_bf[:, 0:1],
                                              start=False, stop=True, skip_group_check=True)
                t_hs[0] = cx.sig("pe", mm)
                pipe.hb_free[T % 2] = t_hs[0]

            def M1(T):
                b = T % 2
                hT = pipe.hT[b]
                cx.wait("pe", *pipe.t_hT[T], t_w)
                cx.wait("act", kst_free[b])
                for j in range(8):
                    pb = mmi[0] % 4
                    cx.wait("pe", mm_free[pb])
                    for kc in range(8):
                        mm = nc.tensor.matmul(psum[pb][:, :], lhsT=wkv[:, kc, j * 128:(j + 1) * 128], rhs=hT[:, kc, :], start=(kc == 0), stop=(kc == 7))
                    t_mm = cx.sig("pe", mm)
                    cx.wait("act", t_mm)
                    t_ev = cx.sig("act", nc.scalar.copy(out=kst[b][:, j, :], in_=psum[pb][:, :]))
                    mm_free[pb] = t_ev
                    mmi[0] += 1
                cx.wait("sp", t_ev)
                kst_free[b] = cx.dma("sp", KT_scr[:, :, T * 512:(T + 1) * 512].rearrange("j p t -> p j t"), kst[b][:], s_kst[b])
                cx.wait("act", vst_free[b], *pre)
                cx.wait("dve", vst_free[b], *pre)
                t_a = t_d = None
                for c in range(4):
                    for hh in range(2):
                        pb = mmi[0] % 4
                        cx.wait("pe", mm_free[pb])
                        for kc in range(8):
                            mm = nc.tensor.matmul(psum[pb][:, :], lhsT=hT[:, kc, c * 128:(c + 1) * 128], rhs=wkv[:, kc, 1024 + hh * 512:1536 + hh * 512],
                                                  start=(kc == 0), stop=(kc == 7))
                        t_mm = cx.sig("pe", mm)
                        if hh == 0:
                            cx.wait("act", t_mm)
                            t_ev = t_a = cx.sig("act", nc.scalar.copy(out=vmst[b][:, :, c, 0:64], in_=psum[pb][:, :].rearrange("p (h e) -> p h e", h=8)))
                        else:
                            cx.wait("dve", t_mm)
                            t_ev = t_d = cx.sig("dve", nc.vector.tensor_copy(out=vdst[b][:, :, c, 0:128], in_=psum[pb][:, :].rearrange("p (h e) -> p h e", h=4)))
                        mm_free[pb] = t_ev
                        mmi[0] += 1
                pipe.hT_free[b] = t_mm
                cx.wait("sp", t_a, t_d)
                cx.dma("sp", VM_scr[:, :, 4 * T:4 * T + 4, :].rearrange("h p c e -> p h (c e)"), vmst[b][:].rearrange("p h c e -> p h (c e)"), s_vst[b])
                vst_free[b] = cx.dma("sp", VD_scr[:, :, 4 * T:4 * T + 4, :].rearrange("h p c e -> p h (c e)"),
                                     vdst[b][:].rearrange("p h c e -> p h (c e)"), s_vst[b])

            for T in range(3):
                pipe.L(T)
            pipe.Q(0); pipe.H(0); pipe.Q(1); pipe.H(1); R1(0)
            for T in range(NTA):
                pipe.L(T + 3)
                pipe.Q(T + 2)
                pipe.H(T + 2)
                R1(T + 1)
                M1(T)
            cx.wait("dve", t_hs[0])
            cx.sig("dve", nc.vector.tensor_copy(out=hs_sb[:], in_=psum[5][:, 0:256].rearrange("p (k b) -> p k b", k=8)))
            cx.barrier()

        with ExitStack() as p2:
            sb2 = lambda name, shape, dtype: p2.enter_context(nc.sbuf_tensor('p2_' + name, shape, dtype))
            qst = [sb2(f"qst{i}", [128, 8, 512], BF16) for i in range(2)]
            gconst = sb2("gconst", [128, 3, 32, 32], F32)
            hbar = sb2("hbar", [128, 8], F32)
            hc = sb2("hc", [128, 8, 32], F32)
            hc_bf = sb2("hc_bf", [128, 8, 32], BF16)
            bd = sb2("bd", [128, 4, 64], BF16)
            gm = sb2("gm", [128, 256], F32)
            top8 = sb2("top8", [128, 8, 8], F32)
            sel = sb2("sel", [128, 256], F32)
            mrow = [sb2(f"mrow{i}", [128, 256], BF16) for i in range(4)]
            mst = [sb2(f"mst{i}", [128, 2, 512], BF16) for i in range(2)]
            t_w = t_wq
            s_g = cx.sem("ld_gc")
            s_qst = [cx.sem("st_q") for _ in range(2)]
            s_mst = [cx.sem("st_m") for _ in range(2)]
            t_gc = cx.dma("sp", gconst[:], gconst_d[:, :, :, :], s_g)
            x_own_v = x_own.rearrange("(t c p) d -> t p c d", c=4, p=128)
            pipe = Pipe(sb2, x_own_v, NTO)
            for T in range(3):
                pipe.L(T)
            t = cx.sig("dve", nc.vector.tensor_reduce(out=hbar[:], in_=hs_sb[:], axis=AX.X, op=ALU.add))
            cx.wait("dve", t)
            t = cx.sig("dve", nc.vector.tensor_scalar(out=hbar[:], in0=hbar[:], scalar1=-1.0 / 32, scalar2=None, op0=ALU.mult))
            cx.wait("dve", t)
            hbar_b = bass.AP(hbar[:].tensor, hbar[:].offset, [hbar[:].ap[0], [1, 8], [0, 32]])
            t = cx.sig("dve", nc.vector.tensor_tensor(out=hc[:], in0=hs_sb[:], in1=hbar_b, op=ALU.add))
            cx.wait("dve", t)
            t_hc = cx.sig("dve", nc.vector.tensor_scalar(out=hc_bf[:], in0=hc[:], scalar1=1.0 / 256, scalar2=None, op0=ALU.mult))
            t_bd0 = cx.sig("dve", nc.vector.memset(bd[:], 0.0))
            cx.wait("pe", t_hc, t_w)
            t_bd = None
            for jp in range(4):
                for kc in range(8):
                    mm = nc.tensor.matmul(psum[jp][:, 0:32], lhsT=wq[:, kc, 1024 + jp * 128:1024 + (jp + 1) * 128], rhs=hc_bf[:, kc, :],
                                          start=(kc == 0), stop=(kc == 7))
                t_mm = cx.sig("pe", mm)
                cx.wait("dve", t_mm, t_bd0)
                cx.sig("dve", nc.vector.tensor_copy(out=bd[0:64, jp, 0:32], in_=psum[jp][0:64, 0:32]))
                t_bd = cx.sig("dve", nc.vector.tensor_copy(out=bd[64:128, jp, 32:64], in_=psum[jp][64:128, 0:32]))
            mm_free = [t_bd] * 4
            qst_free = [None, None]
            mst_free = [None, None]
            mrow_free = [None] * 4
            mmi = [0]

            t_qs = {}
            t_mrs = {}

            def Qpart(n):
                b = n % 2
                hT = pipe.hT[b]
                cx.wait("pe", *pipe.t_hT[n], t_w)
                cx.wait("act", qst_free[b])
                t_q = []
                for j in range(8):
                    pb = mmi[0] % 4
                    cx.wait("pe", mm_free[pb])
                    for kc in range(8):
                        mm = nc.tensor.matmul(psum[pb][:, :], lhsT=wq[:, kc, j * 128:(j + 1) * 128], rhs=hT[:, kc, :], start=(kc == 0), stop=(kc == 7))
                    t_mm = cx.sig("pe", mm)
                    cx.wait("act", t_mm)
                    t_ev = cx.sig("act", nc.scalar.copy(out=qst[b][:, j, :], in_=psum[pb][:, :]))
                    mm_free[pb] = t_ev
                    t_q.append(t_ev)
                    mmi[0] += 1
                pipe.hT_free[b] = t_mm
                t_qs[n] = t_q
                cx.wait("sp", t_q[-1])
                qst_free[b] = cx.dma("sp", QT_scr[:, :, n * 512:(n + 1) * 512].rearrange("j p t -> p j t"), qst[b][:], s_qst[b])

            def Gfront(n):
                if n < 0:
                    return
                b = n % 2
                t_mr_l = []
                for c in range(4):
                    ci = n * 4 + c
                    cx.wait("pe", t_qs[n][3], t_bd, gate_free[0])
                    gb = 4
                    for jp in range(4):
                        mm = nc.tensor.matmul(psum[gb][:, jp * 64:(jp + 1) * 64], lhsT=qst[b][:, jp, c * 128:(c + 1) * 128], rhs=bd[:, jp, :],
                                              start=True, stop=True)
                    t_g = cx.sig("pe", mm)
                    cx.wait("dve", t_g, t_gc)
                    vb_ap = gconst[:, 0, ci, :]
                    vb_b = bass.AP(vb_ap.tensor, vb_ap.offset, [vb_ap.ap[0], [0, 8], vb_ap.ap[1]])
                    va_ap = gconst[:, 1, ci, :]
                    va_b = bass.AP(va_ap.tensor, va_ap.offset, [va_ap.ap[0], [0, 8], va_ap.ap[1]])
                    ow_ap = gconst[:, 2, ci, :]
                    ow_b = bass.AP(ow_ap.tensor, ow_ap.offset, [ow_ap.ap[0], [0, 8], ow_ap.ap[1]])
                    gm3 = gm[:].rearrange("p (h k) -> p h k", h=8)
                    sel3 = sel[:].rearrange("p (h k) -> p h k", h=8)
                    t = cx.sig("dve", nc.vector.tensor_tensor(out=gm3, in0=psum[gb][:, 0:256].rearrange("p (h k) -> p h k", h=8), in1=vb_b, op=ALU.add))
                    gate_free[0] = t
                    cx.wait("dve", t)
                    for hd in range(8):
                        t = cx.sig("dve", nc.vector.max(out=top8[:, hd, :], in_=gm[:, hd * 32:(hd + 1) * 32]))
                    cx.wait("dve", t)
                    t8 = top8[:, :, 2:3]
                    thr_b = bass.AP(t8.tensor, t8.offset, [t8.ap[0], [8, 8], [0, 32]])
                    t = cx.sig("dve", nc.vector.tensor_tensor(out=sel3, in0=gm3, in1=thr_b, op=ALU.is_ge))
                    cx.wait("dve", t)
                    t = cx.sig("dve", nc.vector.tensor_tensor(out=sel3, in0=sel3, in1=va_b, op=ALU.mult))
                    cx.wait("dve", t)
                    t = cx.sig("dve", nc.vector.tensor_tensor(out=sel3, in0=sel3, in1=ow_b, op=ALU.add))
                    cx.wait("dve", t, mrow_free[c])
                    t_mr_l.append(cx.sig("dve", nc.vector.tensor_scalar(out=mrow[c][:], in0=sel[:], scalar1=-1.0, scalar2=-NEG, op0=ALU.add, op1=ALU.mult)))
                t_mrs[n] = t_mr_l

            def Gback(n):
                if n < 0:
                    return
                b = n % 2
                cx.wait("dve", mst_free[b])
                for c in range(4):
                    g2 = c % 2
                    bk = 7 if g2 else 6
                    cx.wait("pe", t_mrs[n][c], tp_free[g2])
                    tp = tp_views[bk]
                    for hf in range(2):
                        tr = nc.tensor.transpose(tp[:, hf * 128:(hf + 1) * 128], mrow[c][:, hf * 128:(hf + 1) * 128], ident[:])
                    t_tp = cx.sig("pe", tr)
                    mrow_free[c] = t_tp
                    cx.wait("dve", t_tp)
                    t_e = cx.sig("dve", nc.vector.tensor_copy(out=mst[b][:, :, c * 128:(c + 1) * 128], in_=tp[:, 0:256].rearrange("p (f t) -> p f t", f=2)))
                    tp_free[g2] = t_e
                cx.wait("sp", t_e)
                mst_free[b] = cx.dma("sp", QM_scr[:, n * 512:(n + 1) * 512].rearrange("(f p) t -> p f t", p=128), mst[b][:], s_mst[b])

            pipe.Q(0); pipe.H(0); pipe.Q(1); pipe.H(1); pipe.R(0)
            for n in range(NTO):
                pipe.L(n + 3)
                pipe.Q(n + 2)
                Gfront(n - 1)
                pipe.R(n + 1)
                pipe.H(n + 2)
                Qpart(n)
                Gback(n - 1)
            Gfront(NTO - 1)
            Gback(NTO - 1)
            cx.barrier()

        p12.close()
        wz = sb("wz", [128, 8, 1024], BF16)
        wo = sb("wo", [128, 8, 1024], BF16)
        s_w4 = cx.sem("ld_w4")
        for i in range(2):
            cx.dma("pool", wz[:, :, i * 512:(i + 1) * 512], w_in_v[:, :, 3072 + i * 512:3072 + (i + 1) * 512], s_w4)
        for i in range(2):
            t_w4 = cx.dma("pool", wo[:, :, i * 512:(i + 1) * 512], w_out_v[:, :, i * 512:(i + 1) * 512], s_w4)
        with ExitStack() as p3:
            sb3 = lambda name, shape, dtype: p3.enter_context(nc.sbuf_tensor('p3_' + name, shape, dtype))
            NSET = 3
            KA = [sb3(f"KA{i}", [128, S], BF16) for i in range(NSET)]
            QA = [sb3(f"QA{i}", [128, S // 2], BF16) for i in range(NSET)]
            VA = [sb3(f"VA{i}", [128, 64 * 129], BF16) for i in range(NSET)]
            cmask = sb3("cmask", [128, 128], BF16)
            PT = [sb3(f"PT{i}", [128, 512], BF16) for i in range(4)]
            n0 = sb3("n0", [128, 32, 128], F32)
            tmpn = sb3("tmpn", [128, 4, 128], F32)
            stage = [sb3(f"stg{i}", [128, 4, 128], BF16) for i in range(2)]
            rden = sb3("rden", [128, 8], F32)
            s_cm = cx.sem("ld_cm")
            s_grp = [cx.sem("ld_grp") for _ in range(NSET)]
            s_stg = [cx.sem("st_stg") for _ in range(2)]
            t_cm = cx.dma("sp", cmask[:], cmask_d[:, :], s_cm)
            for i in range(NSET):
                t_cm = cx.dma("sp", KA[i][64:100, :], kaug_d[:, :], s_cm)
            grp_free = [None] * NSET
            qmask_state = [None] * NSET
            group_order = [7, 0, 6, 1, 5, 2, 14, 3, 15, 4, 12, 13, 8, 9, 10, 11]
            S_banks = [0, 1, 6, 7]
            S_free = [None] * 4
            PT_free = [None] * 4
            acc_free = [None, None]
            stg_free = [None, None]
            u = 0
            rd = 0
            pend = []
            PV_LAG = 3

            def emit_pv(pv):
                cx.wait("pe", pv["t_act"], acc_free[pv["a"]] if pv["first"] else None)
                for (ks, c_lo, c_hi, col) in pv["pieces"]:
                    for c in range(c_lo, c_hi + 1):
                        bank = pv["banks"][c // 2]
                        off = (c % 2) * 129
                        pc = col + (c - c_lo) * 128
                        mm = nc.tensor.matmul(psum[bank][:, off:off + pv["e1"]], lhsT=PT[pv["pt"]][:, pc:pc + 128],
                                              rhs=VA[pv["set"]][:, ks * pv["e1"]:(ks + 1) * pv["e1"]],
                                              start=pv["starts"][(ks, c)], stop=pv["last"], skip_group_check=True)
                t_pv = cx.sig("pe", mm)
                PT_free[pv["pt"]] = t_pv
                if pv["last"]:
                    pv["finish"](t_pv)
                return t_pv

            for gpos, g in enumerate(group_order):
                st = gpos % NSET
                if g < 8:
                    kj, r0, e, gi_q = g // 2, (g % 2) * 64, 64, g
                    vsrc = VM_scr[g]
                    ocol = g * 64
                    dh, mp = None, None
                else:
                    dh, mp = (g - 8) // 2, (g - 8) % 2
                    kj, r0, e, gi_q = 4 + dh, mp * 64, 128, 8 + dh
                    vsrc = VD_scr[dh]
                    ocol = 512 + dh * 128
                e1 = e + 1
                slope = (MOBA_SLOPES + DIFF_SLOPES)[gi_q]
                W = int(np.ceil((ALIBI_THR / slope + 127.0) / 128.0))
                cx.wait("sp", grp_free[st])
                cx.dma("sp", KA[st][0:64, :], KT_scr[kj, r0:r0 + 64, :], s_grp[st])
                cx.dma("sp", QA[st][0:64, :], QT_scr[kj, r0:r0 + 64, :], s_grp[st])
                cx.dma("sp", QA[st][64:68, :], qaug_d[gi_q, :, :], s_grp[st])
                if g < 8:
                    cx.dma("sp", QA[st][68:100, :], QM_scr[g * 32:(g + 1) * 32, :], s_grp[st])
                    qmask_state[st] = "moba"
                elif qmask_state[st] != "zero":
                    cx.dma("sp", QA[st][68:100, :], zrows_d[:, :], s_grp[st])
                    qmask_state[st] = "zero"
                vv = vsrc.rearrange("p kt e -> p (kt e)")
                for q2 in range(2):
                    t_grp = cx.dma("sp", VA[st][:, q2 * 32 * e1:(q2 + 1) * 32 * e1], vv[:, q2 * 32 * e1:(q2 + 1) * 32 * e1], s_grp[st])
                t_last_pe = None
                for n in range(NTO):
                    a = rd % 2
                    banks = (2, 3) if a == 0 else (4, 5)
                    base = 4 * (2 * n + 1)
                    sus = [dict(pieces=[(base + c, c, c, i * 128) for i, c in enumerate([3, 2, 1, 0])], ncols=512, mask=True)]
                    cur = None
                    for ks in range(base + 2, max(-1, base - W), -1):
                        c_lo = max(0, ks - base + 1)
                        c_hi = min(3, ks - base + W - 1)
                        c = c_lo
                        while c <= c_hi:
                            if cur is None or cur["ncols"] == 512:
                                cur = dict(pieces=[], ncols=0, mask=False)
                                sus.append(cur)
                            take = min((512 - cur["ncols"]) // 128, c_hi - c + 1)
                            cur["pieces"].append((ks, c, c + take - 1, cur["ncols"]))
                            cur["ncols"] += take * 128
                            c += take
                    nun = len(sus)
                    bank_started = [False, False]
                    def make_finish(g=g, n=n, a=a, banks=banks, e=e, e1=e1, dh=dh, mp=mp, ocol=ocol):
                        def fin(t_pv):
                            sg_ = a
                            cx.wait("dve", t_pv, stg_free[sg_])
                            for c in range(4):
                                bank = banks[c // 2]
                                off = (c % 2) * 129
                                t = cx.sig("dve", nc.vector.reciprocal(out=rden[:, a * 4 + c:a * 4 + c + 1], in_=psum[bank][:, off + e:off + e + 1]))
                            cx.wait("dve", t)
                            for c in range(4):
                                bank = banks[c // 2]
                                off = (c % 2) * 129
                                sc = rden[:, a * 4 + c:a * 4 + c + 1]
                                src = psum[bank][:, off:off + e]
                                if g < 8:
                                    t = cx.sig("dve", nc.vector.tensor_scalar(out=stage[sg_][:, c, 0:64], in0=src, scalar1=sc, scalar2=None, op0=ALU.mult))
                                elif mp == 0:
                                    t = cx.sig("dve", nc.vector.tensor_scalar(out=n0[:, n * 4 + c, :], in0=src, scalar1=sc, scalar2=None, op0=ALU.mult))
                                else:
                                    t = cx.sig("dve", nc.vector.tensor_scalar(out=tmpn[:, c, :], in0=src, scalar1=sc, scalar2=None, op0=ALU.mult))
                            acc_free[a] = t
                            if g >= 8 and mp == 1:
                                cx.wait("dve", t)
                                for c in range(4):
                                    t = cx.sig("dve", nc.vector.scalar_tensor_tensor(out=stage[sg_][:, c, :], in0=tmpn[:, c, :], scalar=neglam[:, 0:1],
                                                                                     in1=n0[:, n * 4 + c, :], op0=ALU.mult, op1=ALU.add))
                            if g < 8 or mp == 1:
                                cx.wait("pool", t)
                                stg_free[sg_] = cx.dma("pool", att_scr[n * 512:(n + 1) * 512, ocol:ocol + e].rearrange("(c p) f -> p c f", p=128),
                                                       stage[sg_][:, :, 0:e], s_stg[sg_])
                        return fin
                    fin = make_finish()
                    for uu in range(nun):
                        su = sus[uu]
                        ncols = su["ncols"]
                        si = u % 4
                        sbk = S_banks[si]
                        cx.wait("pe", S_free[si], t_grp, t_cm)
                        for (ks, c_lo, c_hi, col) in su["pieces"]:
                            wcol = (c_hi - c_lo + 1) * 128
                            mmq = nc.tensor.matmul(psum[sbk][:, col:col + wcol], lhsT=KA[st][0:KROWS, ks * 128:(ks + 1) * 128],
                                                   rhs=QA[st][0:KROWS, n * 512 + c_lo * 128:n * 512 + (c_hi + 1) * 128], start=True, stop=True)
                        t_s = cx.sig("pe", mmq)
                        if su["mask"]:
                            cx.wait("dve", t_s, t_cm)
                            pv3 = psum[sbk][:, :].rearrange("p (b q) -> p b q", b=4)
                            cm_ap = cmask[:, :]
                            cm_b = bass.AP(cm_ap.tensor, cm_ap.offset, [cm_ap.ap[0], [0, 4], cm_ap.ap[1]])
                            t_s = cx.sig("dve", nc.vector.tensor_tensor(out=pv3, in0=pv3, in1=cm_b, op=ALU.add))
                        pti = u % 4
                        cx.wait("act", t_s, PT_free[pti])
                        t_act = cx.sig("act", nc.scalar.activation(out=PT[pti][:, 0:ncols], in_=psum[sbk][:, 0:ncols], func=AF.Exp, scale=0.125))
                        S_free[si] = t_act
                        starts = {}
                        for (ks, c_lo, c_hi, col) in su["pieces"]:
                            for c in range(c_lo, c_hi + 1):
                                starts[(ks, c)] = not bank_started[c // 2]
                                bank_started[c // 2] = True
                        pend.append(dict(t_act=t_act, a=a, first=(uu == 0), banks=banks, e1=e1, pt=pti, set=st, pieces=su["pieces"],
                                         starts=starts, last=(uu == nun - 1), finish=fin))
                        if len(pend) > PV_LAG:
                            t_last_pe = emit_pv(pend.pop(0))
                        u += 1
                    rd += 1
                while pend:
                    t_last_pe = emit_pv(pend.pop(0))
                grp_free[st] = t_last_pe
            cx.barrier()

        with ExitStack() as p4:
            sb4 = lambda name, shape, dtype: p4.enter_context(nc.sbuf_tensor('p4_' + name, shape, dtype))
            xr = [sb4(f"xr{i}", [128, 4, D], F32) for i in range(2)]
            abuf = [sb4(f"ab{i}", [128, 4, D], BF16) for i in range(2)]
            zs = [sb4(f"zs{i}", [128, 512], F32) for i in range(2)]
            ybuf = sb4("ybuf", [128, 4, D], BF16)
            yT = [sb4(f"yT{i}", [128, 8, 512], BF16) for i in range(2)]
            tmp2 = [sb4(f"tmpo{i}", [128, 512], F32) for i in range(2)]
            ssd = sb4("ssd", [128, 32, 4], F32)
            rsd = sb4("rsd", [128, 32, 4], F32)
            ssf = sb4("ssf", [128, 32], F32)
            rsf = sb4("rsf", [128, 32], F32)
            mh4 = sb4("mh4", [128, 4], F32)
            t_w = t_w4
            s_xr = [cx.sem("ld_xr") for _ in range(2)]
            s_a = [cx.sem("ld_a4") for _ in range(2)]
            s_o = [cx.sem("st_o4") for _ in range(2)]
            t_mh = cx.sig("pool", nc.gpsimd.memset(mh4[:], -0.5))
            cx.wait("dve", t_w)
            gate_k = bass.AP(gate_bc.tensor, gate_bc.offset, [gate_bc.ap[0], [0, 8], gate_bc.ap[1]])
            t_wo = cx.sig("dve", nc.vector.tensor_tensor(out=wo[:], in0=wo[:], in1=gate_k, op=ALU.mult))
            x_own_v = x_own.rearrange("(t c p) d -> t p c d", c=4, p=128)
            att_v = att_scr.rearrange("(t c p) d -> t p c d", c=4, p=128)
            out_v = out_d.rearrange("(t c p) d -> t p c d", c=4, p=128)
            pipe = Pipe(sb4, x_own_v, NTO, nx=2)
            xr_free = [None, None]
            a_free = [None, None]
            t_xr, t_ab = {}, {}
            mm_free = [None] * 4
            zs_free = [None, None]
            t2_free = [None, None]
            y_free = [None]
            yT_free = [None, None]
            t_yTs = {}
            t_ys = {}
            ytp_free = [None, None]
            mmi = [0]
            zi = [0]

            def LAa(n):
                if n >= NTO:
                    return
                b = n % 2
                cx.wait("sp", a_free[b])
                t_ab[n] = cx.dma("sp", abuf[b][:], att_v[n], s_a[b])

            def LAx(n):
                if n >= NTO:
                    return
                b = n % 2
                cx.wait("sp", xr_free[b])
                t_xr[n] = cx.dma("sp", xr[b][:], x_own_v[n], s_xr[b])

            def A4(n):
                if n >= NTO:
                    return
                b = n % 2
                hT = pipe.hT[b]
                t_a = t_ab[n]
                t_dn = []
                for c in range(4):
                    ci = n * 4 + c
                    cx.wait("act", t_a)
                    for dh in range(4):
                        cx.wait("act", junk_tok[0])
                        t = cx.sig("act", nc.scalar.activation(out=junk[:, 0:128], in_=abuf[b][:, c, 512 + dh * 128:512 + (dh + 1) * 128], func=AF.Square,
                                                               accum_out=ssd[:, ci, dh:dh + 1]))
                        junk_tok[0] = t
                    cx.wait("pool", t, t_mh)
                    t = cx.sig("pool", nc.gpsimd.tensor_scalar(out=rsd[:, ci, :], in0=ssd[:, ci, :], scalar1=1.0 / 128, scalar2=EPS, op0=ALU.mult, op1=ALU.add))
                    cx.wait("pool", t)
                    t = cx.sig("pool", nc.gpsimd.tensor_tensor(out=rsd[:, ci, :], in0=rsd[:, ci, :], in1=mh4[:], op=ALU.pow))
                    cx.wait("dve", t, t_a)
                    for dh in range(4):
                        sl = abuf[b][:, c, 512 + dh * 128:512 + (dh + 1) * 128]
                        t = cx.sig("dve", nc.vector.scalar_tensor_tensor(out=sl, in0=sl, scalar=rsd[:, ci, dh:dh + 1], in1=subg_bc[:], op0=ALU.mult, op1=ALU.mult))
                    t_dn.append(t)
                cx.wait("pe", *pipe.t_hT[n], t_w)
                t_y = []
                for c in range(4):
                    for hh in range(2):
                        pb = mmi[0] % 4
                        cx.wait("pe", mm_free[pb])
                        for kc in range(8):
                            mm = nc.tensor.matmul(psum[pb][:, :], lhsT=hT[:, kc, c * 128:(c + 1) * 128], rhs=wz[:, kc, hh * 512:(hh + 1) * 512],
                                                  start=(kc == 0), stop=(kc == 7))
                        t_mm = cx.sig("pe", mm)
                        zb = zi[0] % 2
                        cx.wait("act", t_mm, zs_free[zb])
                        t_z = cx.sig("act", nc.scalar.activation(out=zs[zb][:], in_=psum[pb][:, :], func=AF.Silu))
                        mm_free[pb] = t_z
                        cx.wait("dve", t_z, t_a, t_dn[c], y_free[0])
                        t = cx.sig("dve", nc.vector.tensor_tensor(out=ybuf[:, c, hh * 512:(hh + 1) * 512], in0=abuf[b][:, c, hh * 512:(hh + 1) * 512], in1=zs[zb][:], op=ALU.mult))
                        zs_free[zb] = t
                        mmi[0] += 1
                        zi[0] += 1
                    t_y.append(t)
                pipe.hT_free[b] = t_mm
                a_free[b] = t_y[-1]
                t_ys[n] = t_y

            def A4b(n):
                if n >= NTO:
                    return
                b = n % 2
                t_y = t_ys[n]
                t_yT = []
                t_tps = []
                for c in range(4):
                    bk = (4, 5, 6, 7)[c]
                    cx.wait("pe", t_y[c], ytp_free[c] if c < 2 else tp_free[c - 2])
                    tp = tp_views[bk]
                    for kc in range(8):
                        tr = nc.tensor.transpose(tp[:, kc * 128:(kc + 1) * 128], ybuf[:, c, kc * 128:(kc + 1) * 128], ident[:])
                    t_tps.append(cx.sig("pe", tr))
                t_tp = t_tps[-1]
                for c in range(4):
                    bk = (4, 5, 6, 7)[c]
                    tp = tp_views[bk]
                    cx.wait("act", t_tps[c], yT_free[b])
                    t_e = cx.sig("act", nc.scalar.copy(out=yT[b][:, :, c * 128:(c + 1) * 128], in_=tp[:, :].rearrange("p (k t) -> p k t", k=8)))
                    if c < 2:
                        ytp_free[c] = t_e
                    else:
                        tp_free[c - 2] = t_e
                    t_yT.append(t_e)
                y_free[0] = t_tp
                t_yTs[n] = t_yT

            t_adds = {}

            def B4(n):
                b = n % 2
                t_yT = t_yTs[n]
                adds = []
                for c in range(4):
                    for hh in range(2):
                        pb = mmi[0] % 4
                        cx.wait("pe", mm_free[pb], t_yT[c], t_wo)
                        for kc in range(8):
                            mm = nc.tensor.matmul(psum[pb][:, :], lhsT=yT[b][:, kc, c * 128:(c + 1) * 128], rhs=wo[:, kc, hh * 512:(hh + 1) * 512],
                                                  start=(kc == 0), stop=(kc == 7))
                        t_mm = cx.sig("pe", mm)
                        xs = xr[b][:, c, hh * 512:(hh + 1) * 512]
                        cx.wait("dve", t_mm, t_xr[n])
                        t = cx.sig("dve", nc.vector.tensor_tensor(out=xs, in0=psum[pb][:, :], in1=xs, op=ALU.add))
                        mm_free[pb] = t
                        mmi[0] += 1
                        zi[0] += 1
                    adds.append(t)
                yT_free[b] = t_mm
                t_adds[n] = adds

            def B4ep(n):
                b = n % 2
                for c in range(4):
                    ci = n * 4 + c
                    cx.wait("act", t_adds[n][c])
                    cx.wait("act", junk_tok[0])
                    t = cx.sig("act", nc.scalar.activation(out=junk[:], in_=xr[b][:, c, :], func=AF.Square, accum_out=ssf[:, ci:ci + 1]))
                    junk_tok[0] = t
                    cx.wait("pool", t)
                    t = cx.sig("pool", nc.gpsimd.tensor_scalar(out=rsf[:, ci:ci + 1], in0=ssf[:, ci:ci + 1], scalar1=1.0 / D, scalar2=EPS, op0=ALU.mult, op1=ALU.add))
                    cx.wait("pool", t)
                    t = cx.sig("pool", nc.gpsimd.tensor_tensor(out=rsf[:, ci:ci + 1], in0=rsf[:, ci:ci + 1], in1=mhalf[:], op=ALU.pow))
                    cx.wait("dve", t, t_fing)
                    t = cx.sig("dve", nc.vector.scalar_tensor_tensor(out=xr[b][:, c, :], in0=xr[b][:, c, :], scalar=rsf[:, ci:ci + 1], in1=fing_bc[:],
                                                                     op0=ALU.mult, op1=ALU.mult))
                cx.wait("act", t)
                xr_free[b] = cx.dma("act", out_v[n], xr[b][:], s_o[b])

            for T in range(2):
                pipe.L(T)
            LAa(0)
            LAa(1)
            LAx(0)
            LAx(1)
            pipe.Q(0); pipe.H(0); pipe.Q(1); pipe.H(1); pipe.R(0)
            A4(0)
            A4b(0)
            LAa(2)
            for n in range(NTO):
                pipe.L(n + 2)
                pipe.Q(n + 2)
                pipe.H(n + 2)
                pipe.R(n + 1)
                LAx(n + 1) if n >= 1 else None
                A4(n + 1)
                LAa(n + 3)
                B4(n)
                A4b(n + 1)
                B4ep(n)
            fin_toks = cx.final_dma_toks()
            cx.wait("pool", *fin_toks)
            cx.wait("sp", *fin_toks)
        return nc, cx


def _own_pos(j):
    return np.concatenate([np.arange((2 * n + j) * 512, (2 * n + j + 1) * 512) for n in range(NTO)])


def _consts(j):
    pos = _own_pos(j)
    a_q = (pos // 256).astype(np.float32)
    b_q = (pos % 256).astype(np.float32)
    qaug = np.zeros((12, 4, S // 2), np.float32)
    for i, m in enumerate(MOBA_SLOPES + DIFF_SLOPES):
        qaug[i, 0] = -8.0 * m * 256.0 * a_q
        qaug[i, 1] = -8.0 * m * b_q
        qaug[i, 2] = 8.0 * m
        qaug[i, 3] = 8.0 * m
    lp = np.arange(S)
    gp = lp - 512 * (1 - j)
    real = gp >= 0
    gpc = np.maximum(gp, 0)
    kaug = np.zeros((36, S), np.float32)
    kaug[0] = 1.0
    kaug[1] = 1.0
    kaug[2] = np.where(real, 256.0 * (gpc // 256), -float(2 ** 22))
    kaug[3] = np.where(real, gpc % 256, 0.0)
    kaug[4 + lp[real] // 256, lp[real]] = 1.0
    kk = np.arange(128)[:, None]
    qq = np.arange(128)[None, :]
    cm = np.where(kk > qq, NEG, 0.0).astype(np.float32)
    gc = np.zeros((128, 3, 32, 32), np.float32)
    lb = np.arange(32)
    gb = lb - 2 * (1 - j)
    for ci in range(32):
        cur = pos[ci * 128] // 256
        valid = (gb >= 0) & (gb < cur)
        gc[:, 0, ci, :] = np.where(valid, 0.0, -1e30)[None]
        gc[:, 1, ci, :] = valid.astype(np.float32)[None]
        gc[:, 2, ci, :] = (gb == cur).astype(np.float32)[None]
    return dict(qaug=qaug.astype(BF), kaug=kaug.astype(BF), cmask=cm.astype(BF), gconst=gc,
                ident=np.eye(128, dtype=np.float32).astype(BF), zrows=np.zeros((32, S // 2), BF))


def make_in_maps(inputs):
    f = lambda k: np.ascontiguousarray(np.asarray(inputs[k], dtype=np.float32))
    x = f("x")
    c = f("c")
    shared = dict(
        ln_g=f("ln_g")[0][None, :], b_ada=f("b_ada")[0][None, :], final_g=f("final_g")[None, :], subln_g=f("subln_g")[0][None, :],
        lambda_q1=f("lambda_q1")[0][None, :], lambda_k1=f("lambda_k1")[0][None, :], lambda_q2=f("lambda_q2")[0][None, :],
        lambda_k2=f("lambda_k2")[0][None, :], w_ada=f("w_ada")[0], w_in=f("w_in")[0], w_out=f("w_out")[0])
    consts = [_consts(0), _consts(1)]
    in_maps = []
    for core in range(8):
        b, j = core // 2, core % 2
        m = dict(shared)
        m.update(consts[j])
        if j == 1:
            m["x_all"] = x[b]
        else:
            m["x_all"] = np.concatenate([np.zeros((512, D), np.float32), x[b][:S - 512]], axis=0)
        m["x_own"] = np.ascontiguousarray(x[b][_own_pos(j)])
        m["c_t"] = np.ascontiguousarray(c[b].reshape(8, 128).T)
        in_maps.append(m)
    return in_maps


def kernel(**inputs):
    nc, _ = build_program()
    in_maps = make_in_maps(inputs)
    res = run_bass_kernel_spmd(nc, in_maps, core_ids=list(range(8)))
    out = np.empty((4, S, D), np.float32)
    for core in range(8):
        b, j = core // 2, core % 2
        out[b, _own_pos(j)] = res.results[core]["out"]
    return out
```

```python
import numpy as np
import ml_dtypes
from contextlib import ExitStack
import concourse.bass as bass
import concourse.mybir as mybir
from concourse.alu_op_type import AluOpType as ALU
from concourse.bass_utils import run_bass_kernel_spmd

F32 = mybir.dt.float32
BF16 = mybir.dt.bfloat16
AF = mybir.ActivationFunctionType
AX = mybir.AxisListType
BF = ml_dtypes.bfloat16

S = 8192
D = 1024
NTA = 16
NTO = 8
EPS = 1e-6
NEG = -float(2 ** 18)
KROWS = 100
MOBA_SLOPES = [2.0 ** (-8.0 * (i + 1) / 8) for i in range(8)]
DIFF_SLOPES = [2.0 ** (-8.0 * (i + 1) / 4) for i in range(4)]
LAM_INIT = 0.8 - 0.6 * 1.0
ALIBI_THR = 40.0
DEBUG = False
gate_free = [None]
junk_tok = [None]


class Sem:
    def __init__(self, nc, es, name):
        self.h = es.enter_context(nc.semaphore(name))
        self.n = 0
        self.name = name


class Ctx:
    def __init__(self, nc, es):
        self.nc = nc
        self.es = es
        self.waited = {}
        self.nsem = 0
        self.E = {"pe": nc.tensor, "act": nc.scalar, "dve": nc.vector, "pool": nc.gpsimd, "sp": nc.sync}
        self.prog = {}
        self.last = {}
        self.dma_toks = []
        self.bar = self.sem("bar")
        self.new_prog()

    def sem(self, name):
        self.nsem += 1
        return Sem(self.nc, self.es, f"{name}_{self.nsem}")

    def new_prog(self):
        for k in ["pe", "act", "dve", "pool"]:
            self.prog[k] = self.sem(k + "_prog")

    def wait(self, eng, *toks):
        for t in toks:
            if t is None:
                continue
            if isinstance(t, list):
                self.wait(eng, *t)
                continue
            sem, val = t
            key = (eng, sem.name)
            if self.waited.get(key, 0) >= val:
                continue
            self.E[eng].wait_ge(sem.h, val)
            self.waited[key] = val

    def sig(self, eng, inst):
        sem = self.prog[eng]
        inst.then_inc(sem.h, 1)
        sem.n += 1
        tok = (sem, sem.n)
        self.last[eng] = tok
        return tok

    def dma(self, eng, out, in_, sem, **kw):
        inst = self.E[eng].dma_start(out=out, in_=in_, **kw)
        inst.then_inc(sem.h, 16)
        sem.n += 16
        tok = (sem, sem.n)
        self.dma_toks.append(tok)
        return tok

    def final_dma_toks(self):
        best = {}
        for sem, val in self.dma_toks:
            if sem.name not in best or best[sem.name][1] < val:
                best[sem.name] = (sem, val)
        return list(best.values())

    def barrier(self):
        toks = list(self.last.values()) + self.final_dma_toks()
        self.wait("sp", *toks)
        self.nc.sync.sem_inc(self.bar.h, 1)
        self.bar.n += 1
        for e in ["pe", "act", "dve", "pool"]:
            self.E[e].wait_ge(self.bar.h, self.bar.n)
        self.dma_toks = []
        self.last = {}
        self.new_prog()


def bcast_rows(handle, n):
    return bass.AP(handle, 0, [[0, 128], [1, n]])


def build_program():
    gate_free[0] = None
    junk_tok[0] = None
    nc = bass.Bass("TRN2", target_bir_lowering=False)
    dt = nc.dram_tensor
    x_all = dt("x_all", [S, D], F32, kind="ExternalInput").ap()
    x_own = dt("x_own", [S // 2, D], F32, kind="ExternalInput").ap()
    c_t = dt("c_t", [128, 8], F32, kind="ExternalInput").ap()
    ln_g = dt("ln_g", [1, D], F32, kind="ExternalInput")
    b_ada = dt("b_ada", [1, 3 * D], F32, kind="ExternalInput")
    final_g = dt("final_g", [1, D], F32, kind="ExternalInput")
    subln_g = dt("subln_g", [1, 128], F32, kind="ExternalInput")
    lams = [dt(n, [1, 64], F32, kind="ExternalInput") for n in ("lambda_q1", "lambda_k1", "lambda_q2", "lambda_k2")]
    w_ada = dt("w_ada", [D, 3 * D], F32, kind="ExternalInput").ap()
    w_in = dt("w_in", [D, 4 * D], F32, kind="ExternalInput").ap()
    w_out = dt("w_out", [D, D], F32, kind="ExternalInput").ap()
    ident_d = dt("ident", [128, 128], BF16, kind="ExternalInput").ap()
    kaug_d = dt("kaug", [36, S], BF16, kind="ExternalInput").ap()
    qaug_d = dt("qaug", [12, 4, S // 2], BF16, kind="ExternalInput").ap()
    cmask_d = dt("cmask", [128, 128], BF16, kind="ExternalInput").ap()
    gconst_d = dt("gconst", [128, 3, 32, 32], F32, kind="ExternalInput").ap()
    zrows_d = dt("zrows", [32, S // 2], BF16, kind="ExternalInput").ap()
    out_d = dt("out", [S // 2, D], F32, kind="ExternalOutput").ap()
    skind = "ExternalOutput" if DEBUG else "Internal"
    KT_scr = dt("KT_scr", [8, 128, S], BF16, kind=skind).ap()
    VM_scr = dt("VM_scr", [8, 128, 64, 65], BF16, kind=skind).ap()
    VD_scr = dt("VD_scr", [4, 128, 64, 129], BF16, kind=skind).ap()
    QT_scr = dt("QT_scr", [8, 128, S // 2], BF16, kind=skind).ap()
    QM_scr = dt("QM_scr", [256, S // 2], BF16, kind=skind).ap()
    att_scr = dt("att_scr", [S // 2, D], BF16, kind=skind).ap()

    w_in_v = w_in.rearrange("(kc p) c -> p kc c", p=128)
    w_ada_v = w_ada.rearrange("(kc p) c -> p kc c", p=128)
    w_out_v = w_out.rearrange("(kc p) c -> p kc c", p=128)

    with ExitStack() as es:
        cx = Ctx(nc, es)
        sb = lambda name, shape, dtype: es.enter_context(nc.sbuf_tensor(name, shape, dtype))
        psum = [es.enter_context(nc.psum_tensor(f"ps{i}", [128, 512], F32)) for i in range(8)]
        ident = sb("ident_sb", [128, 128], BF16)
        mod_bc = sb("mod_bc", [128, 3 * D], F32)
        fing_bc = sb("fing_bc", [128, D], F32)
        subg_bc = sb("subg_bc", [128, 128], F32)
        neglam = sb("neglam", [128, 1], F32)
        mhalf = sb("mhalf", [128, 1], F32)
        ones_bf = sb("ones_bf", [128, 1], BF16)
        junk = sb("junk", [128, D], BF16)
        hs_sb = sb("hs_sb", [128, 8, 32], F32)
        shift_bc = mod_bc[:, 0:D]
        geff_bc = mod_bc[:, D:2 * D]
        gate_bc = mod_bc[:, 2 * D:3 * D]
        s_c = cx.sem("ld_const")

        t_ident = cx.dma("sp", ident[:], ident_d[:, :], s_c)
        t_fing = cx.dma("sp", fing_bc[:], bcast_rows(final_g, D), cx.sem("ld_fing"))
        t_subg = cx.dma("sp", subg_bc[:], bcast_rows(subln_g, 128), cx.sem("ld_subg"))
        cx.sig("pool", nc.gpsimd.memset(mhalf[:], -0.5))
        t_ones = cx.sig("pool", nc.gpsimd.memset(ones_bf[:], 1.0))

        p12 = ExitStack()
        wkv = p12.enter_context(nc.sbuf_tensor("wkv_pre", [128, 8, 2048], BF16))
        wq = p12.enter_context(nc.sbuf_tensor("wq_pre", [128, 8, 1536], BF16))
        s_wkv = cx.sem("ld_wkv")
        s_wq = cx.sem("ld_wq")
        for i, c0 in enumerate([512, 2048, 1024, 2560]):
            t_wkv = cx.dma("pool", wkv[:, :, i * 512:(i + 1) * 512], w_in_v[:, :, c0:c0 + 512], s_wkv)
        for i, c0 in enumerate([0, 1536, 512]):
            t_wq = cx.dma("pool", wq[:, :, i * 512:(i + 1) * 512], w_in_v[:, :, c0:c0 + 512], s_wq)
        with ExitStack() as p0:
            sb0 = lambda name, shape, dtype: p0.enter_context(nc.sbuf_tensor('p0_' + name, shape, dtype))
            ct = sb0("ct", [128, 8], F32)
            cact = sb0("cact", [128, 8], F32)
            sg = sb0("sg", [128, 8], F32)
            ones_f = sb0("ones_f", [128, 128], F32)
            crep = sb0("crep", [128, 8, 128], F32)
            bada_bc = sb0("bada_bc", [128, 3 * D], F32)
            lng_bc = sb0("lng_bc", [128, D], F32)
            wa = [sb0(f"wa{i}", [128, 8, 512], F32) for i in range(6)]
            lam_t = [sb0(f"lam{i}", [128, 64], F32) for i in range(4)]
            lprod = sb0("lprod", [128, 2, 64], F32)
            lsum = sb0("lsum", [128, 2], F32)
            lexp = sb0("lexp", [128, 2], F32)
            s_p0 = cx.sem("ld_p0")
            s_wa = [cx.sem("ld_wa") for _ in range(6)]
            t_ct = cx.dma("sp", ct[:], c_t[:, :], s_p0)
            t_bada = cx.dma("sp", bada_bc[:], bcast_rows(b_ada, 3 * D), s_p0)
            t_lng = cx.dma("sp", lng_bc[:], bcast_rows(ln_g, D), s_p0)
            for i in range(4):
                t_lam = cx.dma("sp", lam_t[i][:], bcast_rows(lams[i], 64), s_p0)
            t_p0 = t_lam
            cx.wait("act", t_p0)
            t = cx.sig("act", nc.scalar.activation(out=sg[:], in_=ct[:], func=AF.Sigmoid))
            t_of = cx.sig("dve", nc.vector.memset(ones_f[:], 1.0))
            cx.wait("dve", t, t_p0, t_of)
            t = cx.sig("dve", nc.vector.tensor_tensor(out=cact[:], in0=ct[:], in1=sg[:], op=ALU.mult))
            cx.wait("dve", t)
            for kc in range(8):
                t_crep = cx.sig("dve", nc.vector.tensor_scalar(out=crep[:, kc, :], in0=ones_f[:], scalar1=cact[:, kc:kc + 1],
                                                               scalar2=None, op0=ALU.mult))
            ps_free = [None, None]
            t_was = [cx.dma("sp", wa[cg][:], w_ada_v[:, :, cg * 512:(cg + 1) * 512], s_wa[cg]) for cg in range(6)]
            for cg in range(6):
                b = cg % 2
                cx.wait("pe", t_was[cg], t_crep, ps_free[b])
                for kc in range(8):
                    mm = nc.tensor.matmul(psum[b][:, :], lhsT=crep[:, kc, :], rhs=wa[cg][:, kc, :], start=(kc == 0), stop=(kc == 7))
                t_mm = cx.sig("pe", mm)
                cx.wait("dve", t_mm, t_p0)
                t_ev = cx.sig("dve", nc.vector.tensor_tensor(out=mod_bc[:, cg * 512:(cg + 1) * 512], in0=psum[b][:, :],
                                                             in1=bada_bc[:, cg * 512:(cg + 1) * 512], op=ALU.add))
                ps_free[b] = t_ev
            cx.wait("dve", t_ev)
            t = cx.sig("dve", nc.vector.scalar_tensor_tensor(out=geff_bc, in0=geff_bc, scalar=1.0, in1=lng_bc[:], op0=ALU.add, op1=ALU.mult))
            t1 = cx.sig("dve", nc.vector.tensor_tensor(out=lprod[:, 0, :], in0=lam_t[0][:], in1=lam_t[1][:], op=ALU.mult))
            t2 = cx.sig("dve", nc.vector.tensor_tensor(out=lprod[:, 1, :], in0=lam_t[2][:], in1=lam_t[3][:], op=ALU.mult))
            cx.wait("dve", t2)
            t = cx.sig("dve", nc.vector.tensor_reduce(out=lsum[:], in_=lprod[:], axis=AX.X, op=ALU.add))
            cx.wait("act", t)
            t = cx.sig("act", nc.scalar.activation(out=lexp[:], in_=lsum[:], func=AF.Exp))
            cx.wait("dve", t)
            t = cx.sig("dve", nc.vector.tensor_tensor(out=neglam[:], in0=lexp[:, 1:2], in1=lexp[:, 0:1], op=ALU.subtract))
            cx.wait("dve", t)
            t = cx.sig("dve", nc.vector.tensor_scalar(out=neglam[:], in0=neglam[:], scalar1=-LAM_INIT, scalar2=None, op0=ALU.add))
            cx.wait("dve", t_subg)
            t = cx.sig("dve", nc.vector.tensor_scalar(out=subg_bc[:], in0=subg_bc[:], scalar1=(1.0 - LAM_INIT), scalar2=None, op0=ALU.mult))
            cx.barrier()

        tp_views = {}
        for bk in (4, 5, 6, 7):
            tp_views[bk] = psum[bk][:, :].bitcast(BF16)
        tp_free = {0: None, 1: None}

        class Pipe:
            def __init__(self, alloc, x_v, ntiles, nx=3):
                self.x_v = x_v
                self.nt = ntiles
                self.nx = nx
                self.xb = [alloc(f"xb{i}", [128, 4, D], F32) for i in range(nx)]
                self.hb = [alloc(f"hb{i}", [128, 4, D], BF16) for i in range(2)]
                self.hT = [alloc(f"hT{i}", [128, 8, 512], BF16) for i in range(2)]
                self.tmp = [alloc(f"tmp{i}", [128, D], F32) for i in range(2)]
                self.ssq = alloc("ssq", [128, 4 * ntiles], F32)
                self.rstd = alloc("rstd", [128, 4 * ntiles], F32)
                self.s_x = [cx.sem("ld_x") for _ in range(nx)]
                self.x_free = [None] * nx
                self.hb_free = [None] * 2
                self.hT_free = [None] * 2
                self.t_x, self.t_r, self.t_h, self.t_hT = {}, {}, {}, {}

            def L(self, T):
                if T >= self.nt:
                    return
                b = T % self.nx
                cx.wait("sp", self.x_free[b])
                self.t_x[T] = cx.dma("sp", self.xb[b][:], self.x_v[T], self.s_x[b])

            def Q(self, T):
                if T >= self.nt:
                    return
                xb = self.xb[T % self.nx]
                toks = []
                cx.wait("act", self.t_x[T])
                for c in range(4):
                    col = T * 4 + c
                    cx.wait("act", junk_tok[0])
                    t_ss = cx.sig("act", nc.scalar.activation(out=junk[:], in_=xb[:, c, :], func=AF.Square, accum_out=self.ssq[:, col:col + 1]))
                    junk_tok[0] = t_ss
                    cx.wait("pool", t_ss)
                    t_v = cx.sig("pool", nc.gpsimd.tensor_scalar(out=self.rstd[:, col:col + 1], in0=self.ssq[:, col:col + 1], scalar1=1.0 / D, scalar2=EPS,
                                                                 op0=ALU.mult, op1=ALU.add))
                    cx.wait("pool", t_v)
                    toks.append(cx.sig("pool", nc.gpsimd.tensor_tensor(out=self.rstd[:, col:col + 1], in0=self.rstd[:, col:col + 1], in1=mhalf[:], op=ALU.pow)))
                self.t_r[T] = toks

            def H(self, T):
                if T >= self.nt:
                    return
                xb = self.xb[T % self.nx]
                hb = self.hb[T % 2]
                toks = []
                cx.wait("dve", self.t_x[T], self.hb_free[T % 2])
                for c in range(4):
                    col = T * 4 + c
                    cx.wait("dve", self.t_r[T][c])
                    t_a = cx.sig("dve", nc.vector.scalar_tensor_tensor(out=self.tmp[c % 2][:], in0=xb[:, c, :], scalar=self.rstd[:, col:col + 1], in1=geff_bc,
                                                                       op0=ALU.mult, op1=ALU.mult))
                    cx.wait("dve", t_a)
                    toks.append(cx.sig("dve", nc.vector.tensor_tensor(out=hb[:, c, :], in0=self.tmp[c % 2][:], in1=shift_bc, op=ALU.add)))
                self.t_h[T] = toks
                self.x_free[T % self.nx] = toks[-1]

            def R(self, T):
                if T >= self.nt:
                    return
                hb = self.hb[T % 2]
                hT = self.hT[T % 2]
                toks = []
                for c in range(4):
                    bk = (6, 7)[c % 2]
                    tp = tp_views[bk]
                    cx.wait("pe", self.t_h[T][c], tp_free[c % 2], t_ident)
                    for kc in range(8):
                        tr = nc.tensor.transpose(tp[:, kc * 128:(kc + 1) * 128], hb[:, c, kc * 128:(kc + 1) * 128], ident[:])
                    t_tp = cx.sig("pe", tr)
                    cx.wait("act", t_tp, self.hT_free[T % 2])
                    t_e = cx.sig("act", nc.scalar.copy(out=hT[:, :, c * 128:(c + 1) * 128], in_=tp[:, :].rearrange("p (k t) -> p k t", k=8)))
                    tp_free[c % 2] = t_e
                    toks.append(t_e)
                self.t_hT[T] = toks
                self.hb_free[T % 2] = t_tp

        with ExitStack() as p1:
            sb1 = lambda name, shape, dtype: p1.enter_context(nc.sbuf_tensor('p1_' + name, shape, dtype))
            kst = [sb1(f"kst{i}", [128, 8, 512], BF16) for i in range(2)]
            vmst = [sb1(f"vmst{i}", [128, 8, 4, 65], BF16) for i in range(2)]
            vdst = [sb1(f"vdst{i}", [128, 4, 4, 129], BF16) for i in range(2)]
            s_kst = [cx.sem("st_k") for _ in range(2)]
            s_vst = [cx.sem("st_v") for _ in range(2)]
            t_w = t_wkv
            pre = []
            for i in range(2):
                t1 = cx.sig("dve", nc.vector.memset(vmst[i][:], 1.0))
                t2 = cx.sig("dve", nc.vector.memset(vdst[i][:], 1.0))
                pre = [t1, t2]
            x_all_v = x_all.rearrange("(t c p) d -> t p c d", c=4, p=128)
            pipe = Pipe(sb1, x_all_v, NTA)
            mm_free = [None] * 4
            kst_free = [None, None]
            vst_free = [None, None]
            mmi = [0]
            t_hs = [None]

            def R1(T):
                if T >= NTA:
                    return
                pipe.R(T)
                hb = pipe.hb[T % 2]
                cx.wait("pe", *pipe.t_h[T], t_ones)
                for blk in range(2):
                    for kc in range(8):
                        col = kc * 32 + 2 * T + blk
                        nc.tensor.matmul(psum[5][:, col:col + 1], lhsT=hb[:, 2 * blk, kc * 128:(kc + 1) * 128], rhs=ones_bf[:, 0:1],
                                         start=True, stop=False, skip_group_check=True)
                        mm = nc.tensor.matmul(psum[5][:, col:col + 1], lhsT=hb[:, 2 * blk + 1, kc * 128:(kc + 1) * 128], rhs=ones_bf[:, 0:1],
                                              start=False, stop=True, skip_group_check=True)
                t_hs[0] = cx.sig("pe", mm)
                pipe.hb_free[T % 2] = t_hs[0]

            def M1(T):
                b = T % 2
                hT = pipe.hT[b]
                cx.wait("pe", *pipe.t_hT[T], t_w)
                cx.wait("act", kst_free[b])
                for j in range(8):
                    pb = mmi[0] % 4
                    cx.wait("pe", mm_free[pb])
                    for kc in range(8):
                        mm = nc.tensor.matmul(psum[pb][:, :], lhsT=wkv[:, kc, j * 128:(j + 1) * 128], rhs=hT[:, kc, :], start=(kc == 0), stop=(kc == 7))
                    t_mm = cx.sig("pe", mm)
                    cx.wait("act", t_mm)
                    t_ev = cx.sig("act", nc.scalar.copy(out=kst[b][:, j, :], in_=psum[pb][:, :]))
                    mm_free[pb] = t_ev
                    mmi[0] += 1
                cx.wait("sp", t_ev)
                kst_free[b] = cx.dma("sp", KT_scr[:, :, T * 512:(T + 1) * 512].rearrange("j p t -> p j t"), kst[b][:], s_kst[b])
                cx.wait("act", vst_free[b], *pre)
                cx.wait("dve", vst_free[b], *pre)
                t_a = t_d = None
                for c in range(4):
                    for hh in range(2):
                        pb = mmi[0] % 4
                        cx.wait("pe", mm_free[pb])
                        for kc in range(8):
                            mm = nc.tensor.matmul(psum[pb][:, :], lhsT=hT[:, kc, c * 128:(c + 1) * 128], rhs=wkv[:, kc, 1024 + hh * 512:1536 + hh * 512],
                                                  start=(kc == 0), stop=(kc == 7))
                        t_mm = cx.sig("pe", mm)
                        if hh == 0:
                            cx.wait("act", t_mm)
                            t_ev = t_a = cx.sig("act", nc.scalar.copy(out=vmst[b][:, :, c, 0:64], in_=psum[pb][:, :].rearrange("p (h e) -> p h e", h=8)))
                        else:
                            cx.wait("dve", t_mm)
                            t_ev = t_d = cx.sig("dve", nc.vector.tensor_copy(out=vdst[b][:, :, c, 0:128], in_=psum[pb][:, :].rearrange("p (h e) -> p h e", h=4)))
                        mm_free[pb] = t_ev
                        mmi[0] += 1
                pipe.hT_free[b] = t_mm
                cx.wait("sp", t_a, t_d)
                cx.dma("sp", VM_scr[:, :, 4 * T:4 * T + 4, :].rearrange("h p c e -> p h (c e)"), vmst[b][:].rearrange("p h c e -> p h (c e)"), s_vst[b])
                vst_free[b] = cx.dma("sp", VD_scr[:, :, 4 * T:4 * T + 4, :].rearrange("h p c e -> p h (c e)"),
                                     vdst[b][:].rearrange("p h c e -> p h (c e)"), s_vst[b])

            for T in range(3):
                pipe.L(T)
            pipe.Q(0); pipe.H(0); pipe.Q(1); pipe.H(1); R1(0)
            for T in range(NTA):
                pipe.L(T + 3)
                pipe.Q(T + 2)
                pipe.H(T + 2)
                R1(T + 1)
                M1(T)
            cx.wait("dve", t_hs[0])
            cx.sig("dve", nc.vector.tensor_copy(out=hs_sb[:], in_=psum[5][:, 0:256].rearrange("p (k b) -> p k b", k=8)))
            cx.barrier()

        with ExitStack() as p2:
            sb2 = lambda name, shape, dtype: p2.enter_context(nc.sbuf_tensor('p2_' + name, shape, dtype))
            qst = [sb2(f"qst{i}", [128, 8, 512], BF16) for i in range(2)]
            gconst = sb2("gconst", [128, 3, 32, 32], F32)
            hbar = sb2("hbar", [128, 8], F32)
            hc = sb2("hc", [128, 8, 32], F32)
            hc_bf = sb2("hc_bf", [128, 8, 32], BF16)
            bd = sb2("bd", [128, 4, 64], BF16)
            gm = sb2("gm", [128, 256], F32)
            top8 = sb2("top8", [128, 8, 8], F32)
            sel = sb2("sel", [128, 256], F32)
            mrow = [sb2(f"mrow{i}", [128, 256], BF16) for i in range(4)]
            mst = [sb2(f"mst{i}", [128, 2, 512], BF16) for i in range(2)]
            t_w = t_wq
            s_g = cx.sem("ld_gc")
            s_qst = [cx.sem("st_q") for _ in range(2)]
            s_mst = [cx.sem("st_m") for _ in range(2)]
            t_gc = cx.dma("sp", gconst[:], gconst_d[:, :, :, :], s_g)
            x_own_v = x_own.rearrange("(t c p) d -> t p c d", c=4, p=128)
            pipe = Pipe(sb2, x_own_v, NTO)
            for T in range(3):
                pipe.L(T)
            t = cx.sig("dve", nc.vector.tensor_reduce(out=hbar[:], in_=hs_sb[:], axis=AX.X, op=ALU.add))
            cx.wait("dve", t)
            t = cx.sig("dve", nc.vector.tensor_scalar(out=hbar[:], in0=hbar[:], scalar1=-1.0 / 32, scalar2=None, op0=ALU.mult))
            cx.wait("dve", t)
            hbar_b = bass.AP(hbar[:].tensor, hbar[:].offset, [hbar[:].ap[0], [1, 8], [0, 32]])
            t = cx.sig("dve", nc.vector.tensor_tensor(out=hc[:], in0=hs_sb[:], in1=hbar_b, op=ALU.add))
            cx.wait("dve", t)
            t_hc = cx.sig("dve", nc.vector.tensor_scalar(out=hc_bf[:], in0=hc[:], scalar1=1.0 / 256, scalar2=None, op0=ALU.mult))
            t_bd0 = cx.sig("dve", nc.vector.memset(bd[:], 0.0))
            cx.wait("pe", t_hc, t_w)
            t_bd = None
            for jp in range(4):
                for kc in range(8):
                    mm = nc.tensor.matmul(psum[jp][:, 0:32], lhsT=wq[:, kc, 1024 + jp * 128:1024 + (jp + 1) * 128], rhs=hc_bf[:, kc, :],
                                          start=(kc == 0), stop=(kc == 7))
                t_mm = cx.sig("pe", mm)
                cx.wait("dve", t_mm, t_bd0)
                cx.sig("dve", nc.vector.tensor_copy(out=bd[0:64, jp, 0:32], in_=psum[jp][0:64, 0:32]))
                t_bd = cx.sig("dve", nc.vector.tensor_copy(out=bd[64:128, jp, 32:64], in_=psum[jp][64:128, 0:32]))
            mm_free = [t_bd] * 4
            qst_free = [None, None]
            mst_free = [None, None]
            mrow_free = [None] * 4
            mmi = [0]

            t_qs = {}
            t_mrs = {}

            def Qpart(n):
                b = n % 2
                hT = pipe.hT[b]
                cx.wait("pe", *pipe.t_hT[n], t_w)
                cx.wait("act", qst_free[b])
                t_q = []
                for j in range(8):
                    pb = mmi[0] % 4
                    cx.wait("pe", mm_free[pb])
                    for kc in range(8):
                        mm = nc.tensor.matmul(psum[pb][:, :], lhsT=wq[:, kc, j * 128:(j + 1) * 128], rhs=hT[:, kc, :], start=(kc == 0), stop=(kc == 7))
                    t_mm = cx.sig("pe", mm)
                    cx.wait("act", t_mm)
                    t_ev = cx.sig("act", nc.scalar.copy(out=qst[b][:, j, :], in_=psum[pb][:, :]))
                    mm_free[pb] = t_ev
                    t_q.append(t_ev)
                    mmi[0] += 1
                pipe.hT_free[b] = t_mm
                t_qs[n] = t_q
                cx.wait("sp", t_q[-1])
                qst_free[b] = cx.dma("sp", QT_scr[:, :, n * 512:(n + 1) * 512].rearrange("j p t -> p j t"), qst[b][:], s_qst[b])

            def Gfront(n):
                if n < 0:
                    return
                b = n % 2
                t_mr_l = []
                for c in range(4):
                    ci = n * 4 + c
                    cx.wait("pe", t_qs[n][3], t_bd, gate_free[0])
                    gb = 4
                    for jp in range(4):
                        mm = nc.tensor.matmul(psum[gb][:, jp * 64:(jp + 1) * 64], lhsT=qst[b][:, jp, c * 128:(c + 1) * 128], rhs=bd[:, jp, :],
                                              start=True, stop=True)
                    t_g = cx.sig("pe", mm)
                    cx.wait("dve", t_g, t_gc)
                    vb_ap = gconst[:, 0, ci, :]
                    vb_b = bass.AP(vb_ap.tensor, vb_ap.offset, [vb_ap.ap[0], [0, 8], vb_ap.ap[1]])
                    va_ap = gconst[:, 1, ci, :]
                    va_b = bass.AP(va_ap.tensor, va_ap.offset, [va_ap.ap[0], [0, 8], va_ap.ap[1]])
                    ow_ap = gconst[:, 2, ci, :]
                    ow_b = bass.AP(ow_ap.tensor, ow_ap.offset, [ow_ap.ap[0], [0, 8], ow_ap.ap[1]])
                    gm3 = gm[:].rearrange("p (h k) -> p h k", h=8)
                    sel3 = sel[:].rearrange("p (h k) -> p h k", h=8)
                    t = cx.sig("dve", nc.vector.tensor_tensor(out=gm3, in0=psum[gb][:, 0:256].rearrange("p (h k) -> p h k", h=8), in1=vb_b, op=ALU.add))
                    gate_free[0] = t
                    cx.wait("dve", t)
                    for hd in range(8):
                        t = cx.sig("dve", nc.vector.max(out=top8[:, hd, :], in_=gm[:, hd * 32:(hd + 1) * 32]))
                    cx.wait("dve", t)
                    t8 = top8[:, :, 2:3]
                    thr_b = bass.AP(t8.tensor, t8.offset, [t8.ap[0], [8, 8], [0, 32]])
                    t = cx.sig("dve", nc.vector.tensor_tensor(out=sel3, in0=gm3, in1=thr_b, op=ALU.is_ge))
                    cx.wait("dve", t)
                    t = cx.sig("dve", nc.vector.tensor_tensor(out=sel3, in0=sel3, in1=va_b, op=ALU.mult))
                    cx.wait("dve", t)
                    t = cx.sig("dve", nc.vector.tensor_tensor(out=sel3, in0=sel3, in1=ow_b, op=ALU.add))
                    cx.wait("dve", t, mrow_free[c])
                    t_mr_l.append(cx.sig("dve", nc.vector.tensor_scalar(out=mrow[c][:], in0=sel[:], scalar1=-1.0, scalar2=-NEG, op0=ALU.add, op1=ALU.mult)))
                t_mrs[n] = t_mr_l

            def Gback(n):
                if n < 0:
                    return
                b = n % 2
                cx.wait("dve", mst_free[b])
                for c in range(4):
                    g2 = c % 2
                    bk = 7 if g2 else 6
                    cx.wait("pe", t_mrs[n][c], tp_free[g2])
                    tp = tp_views[bk]
                    for hf in range(2):
                        tr = nc.tensor.transpose(tp[:, hf * 128:(hf + 1) * 128], mrow[c][:, hf * 128:(hf + 1) * 128], ident[:])
                    t_tp = cx.sig("pe", tr)
                    mrow_free[c] = t_tp
                    cx.wait("dve", t_tp)
                    t_e = cx.sig("dve", nc.vector.tensor_copy(out=mst[b][:, :, c * 128:(c + 1) * 128], in_=tp[:, 0:256].rearrange("p (f t) -> p f t", f=2)))
                    tp_free[g2] = t_e
                cx.wait("sp", t_e)
                mst_free[b] = cx.dma("sp", QM_scr[:, n * 512:(n + 1) * 512].rearrange("(f p) t -> p f t", p=128), mst[b][:], s_mst[b])

            pipe.Q(0); pipe.H(0); pipe.Q(1); pipe.H(1); pipe.R(0)
            for n in range(NTO):
                pipe.L(n + 3)
                pipe.Q(n + 2)
                Gfront(n - 1)
                pipe.R(n + 1)
                pipe.H(n + 2)
                Qpart(n)
                Gback(n - 1)
            Gfront(NTO - 1)
            Gback(NTO - 1)
            cx.barrier()

        p12.close()
        wz = sb("wz", [128, 8, 1024], BF16)
        wo = sb("wo", [128, 8, 1024], BF16)
        s_w4 = cx.sem("ld_w4")
        for i in range(2):
            cx.dma("pool", wz[:, :, i * 512:(i + 1) * 512], w_in_v[:, :, 3072 + i * 512:3072 + (i + 1) * 512], s_w4)
        for i in range(2):
            t_w4 = cx.dma("pool", wo[:, :, i * 512:(i + 1) * 512], w_out_v[:, :, i * 512:(i + 1) * 512], s_w4)
        with ExitStack() as p3:
            sb3 = lambda name, shape, dtype: p3.enter_context(nc.sbuf_tensor('p3_' + name, shape, dtype))
            NSET = 3
            KA = [sb3(f"KA{i}", [128, S], BF16) for i in range(NSET)]
            QA = [sb3(f"QA{i}", [128, S // 2], BF16) for i in range(NSET)]
            VA = [sb3(f"VA{i}", [128, 64 * 129], BF16) for i in range(NSET)]
            cmask = sb3("cmask", [128, 128], BF16)
            PT = [sb3(f"PT{i}", [128, 512], BF16) for i in range(4)]
            n0 = sb3("n0", [128, 32, 128], F32)
            tmpn = sb3("tmpn", [128, 4, 128], F32)
            stage = [sb3(f"stg{i}", [128, 4, 128], BF16) for i in range(2)]
            rden = sb3("rden", [128, 8], F32)
            s_cm = cx.sem("ld_cm")
            s_grp = [cx.sem("ld_grp") for _ in range(NSET)]
            s_stg = [cx.sem("st_stg") for _ in range(2)]
            t_cm = cx.dma("sp", cmask[:], cmask_d[:, :], s_cm)
            for i in range(NSET):
                t_cm = cx.dma("sp", KA[i][64:100, :], kaug_d[:, :], s_cm)
            grp_free = [None] * NSET
            qmask_state = [None] * NSET
            group_order = [7, 0, 6, 1, 5, 2, 14, 3, 15, 4, 12, 13, 8, 9, 10, 11]
            S_banks = [0, 1, 6, 7]
            S_free = [None] * 4
            PT_free = [None] * 4
            acc_free = [None, None]
            stg_free = [None, None]
            u = 0
            rd = 0
            pend = []
            PV_LAG = 3

            def emit_pv(pv):
                cx.wait("pe", pv["t_act"], acc_free[pv["a"]] if pv["first"] else None)
                for (ks, c_lo, c_hi, col) in pv["pieces"]:
                    for c in range(c_lo, c_hi + 1):
                        bank = pv["banks"][c // 2]
                        off = (c % 2) * 129
                        pc = col + (c - c_lo) * 128
                        mm = nc.tensor.matmul(psum[bank][:, off:off + pv["e1"]], lhsT=PT[pv["pt"]][:, pc:pc + 128],
                                              rhs=VA[pv["set"]][:, ks * pv["e1"]:(ks + 1) * pv["e1"]],
                                              start=pv["starts"][(ks, c)], stop=pv["last"], skip_group_check=True)
                t_pv = cx.sig("pe", mm)
                PT_free[pv["pt"]] = t_pv
                if pv["last"]:
                    pv["finish"](t_pv)
                return t_pv

            for gpos, g in enumerate(group_order):
                st = gpos % NSET
                if g < 8:
                    kj, r0, e, gi_q = g // 2, (g % 2) * 64, 64, g
                    vsrc = VM_scr[g]
                    ocol = g * 64
                    dh, mp = None, None
                else:
                    dh, mp = (g - 8) // 2, (g - 8) % 2
                    kj, r0, e, gi_q = 4 + dh, mp * 64, 128, 8 + dh
                    vsrc = VD_scr[dh]
                    ocol = 512 + dh * 128
                e1 = e + 1
                slope = (MOBA_SLOPES + DIFF_SLOPES)[gi_q]
                W = int(np.ceil((ALIBI_THR / slope + 127.0) / 128.0))
                cx.wait("sp", grp_free[st])
                cx.dma("sp", KA[st][0:64, :], KT_scr[kj, r0:r0 + 64, :], s_grp[st])
                cx.dma("sp", QA[st][0:64, :], QT_scr[kj, r0:r0 + 64, :], s_grp[st])
                cx.dma("sp", QA[st][64:68, :], qaug_d[gi_q, :, :], s_grp[st])
                if g < 8:
                    cx.dma("sp", QA[st][68:100, :], QM_scr[g * 32:(g + 1) * 32, :], s_grp[st])
                    qmask_state[st] = "moba"
                elif qmask_state[st] != "zero":
                    cx.dma("sp", QA[st][68:100, :], zrows_d[:, :], s_grp[st])
                    qmask_state[st] = "zero"
                vv = vsrc.rearrange("p kt e -> p (kt e)")
                for q2 in range(2):
                    t_grp = cx.dma("sp", VA[st][:, q2 * 32 * e1:(q2 + 1) * 32 * e1], vv[:, q2 * 32 * e1:(q2 + 1) * 32 * e1], s_grp[st])
                t_last_pe = None
                for n in range(NTO):
                    a = rd % 2
                    banks = (2, 3) if a == 0 else (4, 5)
                    base = 4 * (2 * n + 1)
                    sus = [dict(pieces=[(base + c, c, c, i * 128) for i, c in enumerate([3, 2, 1, 0])], ncols=512, mask=True)]
                    cur = None
                    for ks in range(base + 2, max(-1, base - W), -1):
                        c_lo = max(0, ks - base + 1)
                        c_hi = min(3, ks - base + W - 1)
                        c = c_lo
                        while c <= c_hi:
                            if cur is None or cur["ncols"] == 512:
                                cur = dict(pieces=[], ncols=0, mask=False)
                                sus.append(cur)
                            take = min((512 - cur["ncols"]) // 128, c_hi - c + 1)
                            cur["pieces"].append((ks, c, c + take - 1, cur["ncols"]))
                            cur["ncols"] += take * 128
                            c += take
                    nun = len(sus)
                    bank_started = [False, False]
                    def make_finish(g=g, n=n, a=a, banks=banks, e=e, e1=e1, dh=dh, mp=mp, ocol=ocol):
                        def fin(t_pv):
                            sg_ = a
                            cx.wait("dve", t_pv, stg_free[sg_])
                            for c in range(4):
                                bank = banks[c // 2]
                                off = (c % 2) * 129
                                t = cx.sig("dve", nc.vector.reciprocal(out=rden[:, a * 4 + c:a * 4 + c + 1], in_=psum[bank][:, off + e:off + e + 1]))
                            cx.wait("dve", t)
                            for c in range(4):
                                bank = banks[c // 2]
                                off = (c % 2) * 129
                                sc = rden[:, a * 4 + c:a * 4 + c + 1]
                                src = psum[bank][:, off:off + e]
                                if g < 8:
                                    t = cx.sig("dve", nc.vector.tensor_scalar(out=stage[sg_][:, c, 0:64], in0=src, scalar1=sc, scalar2=None, op0=ALU.mult))
                                elif mp == 0:
                                    t = cx.sig("dve", nc.vector.tensor_scalar(out=n0[:, n * 4 + c, :], in0=src, scalar1=sc, scalar2=None, op0=ALU.mult))
                                else:
                                    t = cx.sig("dve", nc.vector.tensor_scalar(out=tmpn[:, c, :], in0=src, scalar1=sc, scalar2=None, op0=ALU.mult))
                            acc_free[a] = t
                            if g >= 8 and mp == 1:
                                cx.wait("dve", t)
                                for c in range(4):
                                    t = cx.sig("dve", nc.vector.scalar_tensor_tensor(out=stage[sg_][:, c, :], in0=tmpn[:, c, :], scalar=neglam[:, 0:1],
                                                                                     in1=n0[:, n * 4 + c, :], op0=ALU.mult, op1=ALU.add))
                            if g < 8 or mp == 1:
                                cx.wait("pool", t)
                                stg_free[sg_] = cx.dma("pool", att_scr[n * 512:(n + 1) * 512, ocol:ocol + e].rearrange("(c p) f -> p c f", p=128),
                                                       stage[sg_][:, :, 0:e], s_stg[sg_])
                        return fin
                    fin = make_finish()
                    for uu in range(nun):
                        su = sus[uu]
                        ncols = su["ncols"]
                        si = u % 4
                        sbk = S_banks[si]
                        cx.wait("pe", S_free[si], t_grp, t_cm)
                        for (ks, c_lo, c_hi, col) in su["pieces"]:
                            wcol = (c_hi - c_lo + 1) * 128
                            mmq = nc.tensor.matmul(psum[sbk][:, col:col + wcol], lhsT=KA[st][0:KROWS, ks * 128:(ks + 1) * 128],
                                                   rhs=QA[st][0:KROWS, n * 512 + c_lo * 128:n * 512 + (c_hi + 1) * 128], start=True, stop=True)
                        t_s = cx.sig("pe", mmq)
                        if su["mask"]:
                            cx.wait("dve", t_s, t_cm)
                            pv3 = psum[sbk][:, :].rearrange("p (b q) -> p b q", b=4)
                            cm_ap = cmask[:, :]
                            cm_b = bass.AP(cm_ap.tensor, cm_ap.offset, [cm_ap.ap[0], [0, 4], cm_ap.ap[1]])
                            t_s = cx.sig("dve", nc.vector.tensor_tensor(out=pv3, in0=pv3, in1=cm_b, op=ALU.add))
                        pti = u % 4
                        cx.wait("act", t_s, PT_free[pti])
                        t_act = cx.sig("act", nc.scalar.activation(out=PT[pti][:, 0:ncols], in_=psum[sbk][:, 0:ncols], func=AF.Exp, scale=0.125))
                        S_free[si] = t_act
                        starts = {}
                        for (ks, c_lo, c_hi, col) in su["pieces"]:
                            for c in range(c_lo, c_hi + 1):
                                starts[(ks, c)] = not bank_started[c // 2]
                                bank_started[c // 2] = True
                        pend.append(dict(t_act=t_act, a=a, first=(uu == 0), banks=banks, e1=e1, pt=pti, set=st, pieces=su["pieces"],
                                         starts=starts, last=(uu == nun - 1), finish=fin))
                        if len(pend) > PV_LAG:
                            t_last_pe = emit_pv(pend.pop(0))
                        u += 1
                    rd += 1
                while pend:
                    t_last_pe = emit_pv(pend.pop(0))
                grp_free[st] = t_last_pe
            cx.barrier()

        with ExitStack() as p4:
            sb4 = lambda name, shape, dtype: p4.enter_context(nc.sbuf_tensor('p4_' + name, shape, dtype))
            xr = [sb4(f"xr{i}", [128, 4, D], F32) for i in range(2)]
            abuf = [sb4(f"ab{i}", [128, 4, D], BF16) for i in range(2)]
            zs = [sb4(f"zs{i}", [128, 512], F32) for i in range(2)]
            ybuf = sb4("ybuf", [128, 4, D], BF16)
            yT = [sb4(f"yT{i}", [128, 8, 512], BF16) for i in range(2)]
            tmp2 = [sb4(f"tmpo{i}", [128, 512], F32) for i in range(2)]
            ssd = sb4("ssd", [128, 32, 4], F32)
            rsd = sb4("rsd", [128, 32, 4], F32)
            ssf = sb4("ssf", [128, 32], F32)
            rsf = sb4("rsf", [128, 32], F32)
            mh4 = sb4("mh4", [128, 4], F32)
            t_w = t_w4
            s_xr = [cx.sem("ld_xr") for _ in range(2)]
            s_a = [cx.sem("ld_a4") for _ in range(2)]
            s_o = [cx.sem("st_o4") for _ in range(2)]
            t_mh = cx.sig("pool", nc.gpsimd.memset(mh4[:], -0.5))
            cx.wait("dve", t_w)
            gate_k = bass.AP(gate_bc.tensor, gate_bc.offset, [gate_bc.ap[0], [0, 8], gate_bc.ap[1]])
            t_wo = cx.sig("dve", nc.vector.tensor_tensor(out=wo[:], in0=wo[:], in1=gate_k, op=ALU.mult))
            x_own_v = x_own.rearrange("(t c p) d -> t p c d", c=4, p=128)
            att_v = att_scr.rearrange("(t c p) d -> t p c d", c=4, p=128)
            out_v = out_d.rearrange("(t c p) d -> t p c d", c=4, p=128)
            pipe = Pipe(sb4, x_own_v, NTO, nx=2)
            xr_free = [None, None]
            a_free = [None, None]
            t_xr, t_ab = {}, {}
            mm_free = [None] * 4
            zs_free = [None, None]
            t2_free = [None, None]
            y_free = [None]
            yT_free = [None, None]
            t_yTs = {}
            t_ys = {}
            ytp_free = [None, None]
            mmi = [0]
            zi = [0]

            def LAa(n):
                if n >= NTO:
                    return
                b = n % 2
                cx.wait("sp", a_free[b])
                t_ab[n] = cx.dma("sp", abuf[b][:], att_v[n], s_a[b])

            def LAx(n):
                if n >= NTO:
                    return
                b = n % 2
                cx.wait("sp", xr_free[b])
                t_xr[n] = cx.dma("sp", xr[b][:], x_own_v[n], s_xr[b])

            def A4(n):
                if n >= NTO:
                    return
                b = n % 2
                hT = pipe.hT[b]
                t_a = t_ab[n]
                t_dn = []
                for c in range(4):
                    ci = n * 4 + c
                    cx.wait("act", t_a)
                    for dh in range(4):
                        cx.wait("act", junk_tok[0])
                        t = cx.sig("act", nc.scalar.activation(out=junk[:, 0:128], in_=abuf[b][:, c, 512 + dh * 128:512 + (dh + 1) * 128], func=AF.Square,
                                                               accum_out=ssd[:, ci, dh:dh + 1]))
                        junk_tok[0] = t
                    cx.wait("pool", t, t_mh)
                    t = cx.sig("pool", nc.gpsimd.tensor_scalar(out=rsd[:, ci, :], in0=ssd[:, ci, :], scalar1=1.0 / 128, scalar2=EPS, op0=ALU.mult, op1=ALU.add))
                    cx.wait("pool", t)
                    t = cx.sig("pool", nc.gpsimd.tensor_tensor(out=rsd[:, ci, :], in0=rsd[:, ci, :], in1=mh4[:], op=ALU.pow))
                    cx.wait("dve", t, t_a)
                    for dh in range(4):
                        sl = abuf[b][:, c, 512 + dh * 128:512 + (dh + 1) * 128]
                        t = cx.sig("dve", nc.vector.scalar_tensor_tensor(out=sl, in0=sl, scalar=rsd[:, ci, dh:dh + 1], in1=subg_bc[:], op0=ALU.mult, op1=ALU.mult))
                    t_dn.append(t)
                cx.wait("pe", *pipe.t_hT[n], t_w)
                t_y = []
                for c in range(4):
                    for hh in range(2):
                        pb = mmi[0] % 4
                        cx.wait("pe", mm_free[pb])
                        for kc in range(8):
                            mm = nc.tensor.matmul(psum[pb][:, :], lhsT=hT[:, kc, c * 128:(c + 1) * 128], rhs=wz[:, kc, hh * 512:(hh + 1) * 512],
                                                  start=(kc == 0), stop=(kc == 7))
                        t_mm = cx.sig("pe", mm)
                        zb = zi[0] % 2
                        cx.wait("act", t_mm, zs_free[zb])
                        t_z = cx.sig("act", nc.scalar.activation(out=zs[zb][:], in_=psum[pb][:, :], func=AF.Silu))
                        mm_free[pb] = t_z
                        cx.wait("dve", t_z, t_a, t_dn[c], y_free[0])
                        t = cx.sig("dve", nc.vector.tensor_tensor(out=ybuf[:, c, hh * 512:(hh + 1) * 512], in0=abuf[b][:, c, hh * 512:(hh + 1) * 512], in1=zs[zb][:], op=ALU.mult))
                        zs_free[zb] = t
                        mmi[0] += 1
                        zi[0] += 1
                    t_y.append(t)
                pipe.hT_free[b] = t_mm
                a_free[b] = t_y[-1]
                t_ys[n] = t_y

            def A4b(n):
                if n >= NTO:
                    return
                b = n % 2
                t_y = t_ys[n]
                t_yT = []
                t_tps = []
                for c in range(4):
                    bk = (4, 5, 6, 7)[c]
                    cx.wait("pe", t_y[c], ytp_free[c] if c < 2 else tp_free[c - 2])
                    tp = tp_views[bk]
                    for kc in range(8):
                        tr = nc.tensor.transpose(tp[:, kc * 128:(kc + 1) * 128], ybuf[:, c, kc * 128:(kc + 1) * 128], ident[:])
                    t_tps.append(cx.sig("pe", tr))
                t_tp = t_tps[-1]
                for c in range(4):
                    bk = (4, 5, 6, 7)[c]
                    tp = tp_views[bk]
                    cx.wait("act", t_tps[c], yT_free[b])
                    t_e = cx.sig("act", nc.scalar.copy(out=yT[b][:, :, c * 128:(c + 1) * 128], in_=tp[:, :].rearrange("p (k t) -> p k t", k=8)))
                    if c < 2:
                        ytp_free[c] = t_e
                    else:
                        tp_free[c - 2] = t_e
                    t_yT.append(t_e)
                y_free[0] = t_tp
                t_yTs[n] = t_yT

            t_adds = {}

            def B4(n):
                b = n % 2
                t_yT = t_yTs[n]
                adds = []
                for c in range(4):
                    for hh in range(2):
                        pb = mmi[0] % 4
                        cx.wait("pe", mm_free[pb], t_yT[c], t_wo)
                        for kc in range(8):
                            mm = nc.tensor.matmul(psum[pb][:, :], lhsT=yT[b][:, kc, c * 128:(c + 1) * 128], rhs=wo[:, kc, hh * 512:(hh + 1) * 512],
                                                  start=(kc == 0), stop=(kc == 7))
                        t_mm = cx.sig("pe", mm)
                        xs = xr[b][:, c, hh * 512:(hh + 1) * 512]
                        cx.wait("dve", t_mm, t_xr[n])
                        t = cx.sig("dve", nc.vector.tensor_tensor(out=xs, in0=psum[pb][:, :], in1=xs, op=ALU.add))
                        mm_free[pb] = t
                        mmi[0] += 1
                        zi[0] += 1
                    adds.append(t)
                yT_free[b] = t_mm
                t_adds[n] = adds

            def B4ep(n):
                b = n % 2
                for c in range(4):
                    ci = n * 4 + c
                    cx.wait("act", t_adds[n][c])
                    cx.wait("act", junk_tok[0])
                    t = cx.sig("act", nc.scalar.activation(out=junk[:], in_=xr[b][:, c, :], func=AF.Square, accum_out=ssf[:, ci:ci + 1]))
                    junk_tok[0] = t
                    cx.wait("pool", t)
                    t = cx.sig("pool", nc.gpsimd.tensor_scalar(out=rsf[:, ci:ci + 1], in0=ssf[:, ci:ci + 1], scalar1=1.0 / D, scalar2=EPS, op0=ALU.mult, op1=ALU.add))
                    cx.wait("pool", t)
                    t = cx.sig("pool", nc.gpsimd.tensor_tensor(out=rsf[:, ci:ci + 1], in0=rsf[:, ci:ci + 1], in1=mhalf[:], op=ALU.pow))
                    cx.wait("dve", t, t_fing)
                    t = cx.sig("dve", nc.vector.scalar_tensor_tensor(out=xr[b][:, c, :], in0=xr[b][:, c, :], scalar=rsf[:, ci:ci + 1], in1=fing_bc[:],
                                                                     op0=ALU.mult, op1=ALU.mult))
                cx.wait("pool", t)
                xr_free[b] = cx.dma("pool", out_v[n], xr[b][:], s_o[b])

            for T in range(2):
                pipe.L(T)
            LAa(0)
            LAa(1)
            LAx(0)
            LAx(1)
            pipe.Q(0); pipe.H(0); pipe.Q(1); pipe.H(1); pipe.R(0)
            A4(0)
            A4b(0)
            LAa(2)
            for n in range(NTO):
                pipe.L(n + 2)
                pipe.Q(n + 2)
                pipe.H(n + 2)
                pipe.R(n + 1)
                LAx(n + 1) if n >= 1 else None
                A4(n + 1)
                LAa(n + 3)
                B4(n)
                A4b(n + 1)
                B4ep(n)
            fin_toks = cx.final_dma_toks()
            cx.wait("pool", *fin_toks)
            cx.wait("sp", *fin_toks)
        return nc, cx


def _own_pos(j):
    return np.concatenate([np.arange((2 * n + j) * 512, (2 * n + j + 1) * 512) for n in range(NTO)])


def _consts(j):
    pos = _own_pos(j)
    a_q = (pos // 256).astype(np.float32)
    b_q = (pos % 256).astype(np.float32)
    qaug = np.zeros((12, 4, S // 2), np.float32)
    for i, m in enumerate(MOBA_SLOPES + DIFF_SLOPES):
        qaug[i, 0] = -8.0 * m * 256.0 * a_q
        qaug[i, 1] = -8.0 * m * b_q
        qaug[i, 2] = 8.0 * m
        qaug[i, 3] = 8.0 * m
    lp = np.arange(S)
    gp = lp - 512 * (1 - j)
    real = gp >= 0
    gpc = np.maximum(gp, 0)
    kaug = np.zeros((36, S), np.float32)
    kaug[0] = 1.0
    kaug[1] = 1.0
    kaug[2] = np.where(real, 256.0 * (gpc // 256), -float(2 ** 22))
    kaug[3] = np.where(real, gpc % 256, 0.0)
    kaug[4 + lp[real] // 256, lp[real]] = 1.0
    kk = np.arange(128)[:, None]
    qq = np.arange(128)[None, :]
    cm = np.where(kk > qq, NEG, 0.0).astype(np.float32)
    gc = np.zeros((128, 3, 32, 32), np.float32)
    lb = np.arange(32)
    gb = lb - 2 * (1 - j)
    for ci in range(32):
        cur = pos[ci * 128] // 256
        valid = (gb >= 0) & (gb < cur)
        gc[:, 0, ci, :] = np.where(valid, 0.0, -1e30)[None]
        gc[:, 1, ci, :] = valid.astype(np.float32)[None]
        gc[:, 2, ci, :] = (gb == cur).astype(np.float32)[None]
    return dict(qaug=qaug.astype(BF), kaug=kaug.astype(BF), cmask=cm.astype(BF), gconst=gc,
                ident=np.eye(128, dtype=np.float32).astype(BF), zrows=np.zeros((32, S // 2), BF))


def make_in_maps(inputs):
    f = lambda k: np.ascontiguousarray(np.asarray(inputs[k], dtype=np.float32))
    x = f("x")
    c = f("c")
    shared = dict(
        ln_g=f("ln_g")[0][None, :], b_ada=f("b_ada")[0][None, :], final_g=f("final_g")[None, :], subln_g=f("subln_g")[0][None, :],
        lambda_q1=f("lambda_q1")[0][None, :], lambda_k1=f("lambda_k1")[0][None, :], lambda_q2=f("lambda_q2")[0][None, :],
        lambda_k2=f("lambda_k2")[0][None, :], w_ada=f("w_ada")[0], w_in=f("w_in")[0], w_out=f("w_out")[0])
    consts = [_consts(0), _consts(1)]
    in_maps = []
    for core in range(8):
        b, j = core // 2, core % 2
        m = dict(shared)
        m.update(consts[j])
        if j == 1:
            m["x_all"] = x[b]
        else:
            m["x_all"] = np.concatenate([np.zeros((512, D), np.float32), x[b][:S - 512]], axis=0)
        m["x_own"] = np.ascontiguousarray(x[b][_own_pos(j)])
        m["c_t"] = np.ascontiguousarray(c[b].reshape(8, 128).T)
        in_maps.append(m)
    return in_maps


def kernel(**inputs):
    nc, _ = build_program()
    in_maps = make_in_maps(inputs)
    res = run_bass_kernel_spmd(nc, in_maps, core_ids=list(range(8)))
    out = np.empty((4, S, D), np.float32)
    for core in range(8):
        b, j = core // 2, core % 2
        out[b, _own_pos(j)] = res.results[core]["out"]
    return out
```
